# Optimizing a Trainium2 kernel written in Bass

```python
import jax, jax.numpy as jnp
from jax import lax
import numpy as np

D_MODEL = 1024
BATCH = 8
SEQ = 4096
DEPTH = 2

GRID_W = 64
NA_HEADS = 8
NA_HEAD_DIM = 64
NA_WIN_ROWS = 8
NA_WIN_COLS = 16
NA_WIDTH = NA_HEADS * NA_HEAD_DIM
MLA_HEADS = 8
MLA_Q_RANK = 384
MLA_KV_RANK = 256
MLA_NOPE_DIM = 64
MLA_ROPE_DIM = 32
MLA_V_DIM = 64
MLA_WIDTH = MLA_HEADS * MLA_V_DIM
ROPE_BASE = 10000.0
Q_BLOCK = 128
N_EXPERTS = 16
EC_CAPACITY = 2
EXPERT_FF = 1024
PLE_DIM = 256
RMS_EPS = 1e-6
IN_SPLITS = [NA_WIDTH, NA_WIDTH, NA_WIDTH, MLA_Q_RANK, MLA_KV_RANK, MLA_ROPE_DIM, D_MODEL, D_MODEL]
IN_COLS = sum(IN_SPLITS)
IN_CUTS = [int(c) for c in np.cumsum(IN_SPLITS)[:-1]]

kernel_name = "hybrid_na_mla_ec_moe_encoder"


def rmsnorm(x, g):
    xf = x.astype(jnp.float32)
    y = xf * lax.rsqrt(jnp.mean(xf * xf, axis=-1, keepdims=True) + RMS_EPS)
    return (y * g.astype(jnp.float32)).astype(x.dtype)


def rope(x, pos):
    dim = x.shape[-1]
    half = dim // 2
    freqs = 1.0 / (ROPE_BASE ** (jnp.arange(0, dim, 2, dtype=jnp.float32) / dim))
    ang = pos.astype(jnp.float32)[:, None] * freqs[None, :]
    shape = (ang.shape[0],) + (1,) * (x.ndim - 3) + (half,)
    cos = jnp.cos(ang).reshape(shape).astype(x.dtype)
    sin = jnp.sin(ang).reshape(shape).astype(x.dtype)
    x1, x2 = x[..., :half], x[..., half:]
    return jnp.concatenate([x1 * cos - x2 * sin, x2 * cos + x1 * sin], axis=-1)


def rope2d(x, rows_pos, cols_pos):
    half = x.shape[-1] // 2
    return jnp.concatenate([rope(x[..., :half], rows_pos), rope(x[..., half:], cols_pos)], axis=-1)


def neighbourhood_attention(q, k, v, rpb):
    B, S, H, d = q.shape
    rows = S // GRID_W
    kh = min(NA_WIN_ROWS, rows)
    kw = NA_WIN_COLS
    scale = d ** -0.5
    qg = q.reshape(B, rows, GRID_W, H, d).transpose(1, 0, 2, 3, 4)
    kg = k.reshape(B, rows, GRID_W, H, d)
    vg = v.reshape(B, rows, GRID_W, H, d)
    col = jnp.arange(GRID_W)
    col_start = jnp.clip(col - kw // 2, 0, GRID_W - kw)
    col_idx = col_start[:, None] + jnp.arange(kw)[None, :]
    col_bias_idx = col_idx - col[:, None] + (NA_WIN_COLS - 1)

    def row_block(args):
        r, q_row = args
        r0 = jnp.clip(r - kh // 2, 0, rows - kh)
        k_rows = lax.dynamic_slice_in_dim(kg, r0, kh, axis=1)
        v_rows = lax.dynamic_slice_in_dim(vg, r0, kh, axis=1)
        k_win = k_rows[:, :, col_idx]
        v_win = v_rows[:, :, col_idx]
        row_bias_idx = r0 + jnp.arange(kh) - r + (NA_WIN_ROWS - 1)
        bias = rpb[:, row_bias_idx][:, :, col_bias_idx]
        bias = bias.transpose(0, 2, 1, 3).astype(jnp.float32)
        s = jnp.einsum('bqhd,bkqjhd->bhqkj', q_row, k_win).astype(jnp.float32) * scale + bias[None]
        pr = jax.nn.softmax(s.reshape(B, H, GRID_W, kh * kw), axis=-1)
        pr = pr.reshape(B, H, GRID_W, kh, kw).astype(v.dtype)
        return jnp.einsum('bhqkj,bkqjhd->bqhd', pr, v_win)

    out = lax.map(row_block, (jnp.arange(rows), qg))
    return out.transpose(1, 0, 2, 3, 4).reshape(B, S, H * d)


def latent_attention(q_lat, kv_lat, k_rope, q_norm, wq_up, kv_norm, wkv_up, rows_pos, cols_pos):
    B, S, _ = q_lat.shape
    H = MLA_HEADS
    q = (rmsnorm(q_lat, q_norm) @ wq_up).reshape(B, S, H, MLA_NOPE_DIM + MLA_ROPE_DIM)
    q_nope, q_pe = q[..., :MLA_NOPE_DIM], q[..., MLA_NOPE_DIM:]
    q_pe = rope2d(q_pe, rows_pos, cols_pos)
    kv = (rmsnorm(kv_lat, kv_norm) @ wkv_up).reshape(B, S, H, MLA_NOPE_DIM + MLA_V_DIM)
    k_nope, v = kv[..., :MLA_NOPE_DIM], kv[..., MLA_NOPE_DIM:]
    k_pe = rope2d(k_rope, rows_pos, cols_pos)
    k = jnp.concatenate([k_nope, jnp.broadcast_to(k_pe[:, :, None, :], (B, S, H, MLA_ROPE_DIM))], axis=-1)
    qf = jnp.concatenate([q_nope, q_pe], axis=-1)
    dqk = MLA_NOPE_DIM + MLA_ROPE_DIM
    scale = dqk ** -0.5
    nb = S // Q_BLOCK
    qb = qf.reshape(B, nb, Q_BLOCK, H, dqk).transpose(1, 0, 2, 3, 4)

    def block(qblk):
        s = jnp.einsum('bqhd,bkhd->bhqk', qblk, k).astype(jnp.float32) * scale
        pr = jax.nn.softmax(s, axis=-1).astype(v.dtype)
        return jnp.einsum('bhqk,bkhv->bqhv', pr, v)

    out = lax.map(block, qb)
    return out.transpose(1, 0, 2, 3, 4).reshape(B, S, H * MLA_V_DIM)


def expert_choice_moe(h, w_router, w1, w3, w2):
    B, S, D = h.shape
    cap = EC_CAPACITY * S // N_EXPERTS
    logits = jnp.einsum('bsd,de->bse', h, w_router).astype(jnp.float32)
    aff = jax.nn.softmax(logits, axis=-1)
    vals, idx = lax.top_k(aff.transpose(0, 2, 1), cap)
    bidx = jnp.arange(B)[:, None, None]
    xg = h[bidx, idx]
    h1 = jnp.einsum('becd,edf->becf', xg, w1)
    h3 = jnp.einsum('becd,edf->becf', xg, w3)
    out = jnp.einsum('becf,efd->becd', jax.nn.silu(h1) * h3, w2)
    out = out * vals.astype(h.dtype)[..., None]
    return jnp.zeros_like(h).at[bidx, idx].add(out)


def setup_inputs(seed: int = 0) -> dict:
    key = jax.random.key(seed)
    ks = jax.random.split(key, 24)

    def nrm(k, shape, scale):
        return jax.random.normal(k, shape, jnp.float32) * scale

    def gain(k, shape):
        return 1.0 + 0.01 * jax.random.normal(k, shape, jnp.float32)

    L, D = DEPTH, D_MODEL
    return {
        "x": nrm(ks[0], (BATCH, SEQ, D), 1.0),
        "p": nrm(ks[1], (DEPTH, BATCH, SEQ, PLE_DIM), 1.0),
        "norm_mix": gain(ks[2], (L, D)),
        "w_in": nrm(ks[3], (L, D, IN_COLS), D ** -0.5),
        "na_rpb": nrm(ks[4], (L, NA_HEADS, 2 * NA_WIN_ROWS - 1, 2 * NA_WIN_COLS - 1), 0.02),
        "mla_q_norm": gain(ks[5], (L, MLA_Q_RANK)),
        "mla_wq_up": nrm(ks[6], (L, MLA_Q_RANK, MLA_HEADS * (MLA_NOPE_DIM + MLA_ROPE_DIM)), MLA_Q_RANK ** -0.5),
        "mla_kv_norm": gain(ks[7], (L, MLA_KV_RANK)),
        "mla_wkv_up": nrm(ks[8], (L, MLA_KV_RANK, MLA_HEADS * (MLA_NOPE_DIM + MLA_V_DIM)), MLA_KV_RANK ** -0.5),
        "w_na_o": nrm(ks[9], (L, NA_WIDTH, D), NA_WIDTH ** -0.5),
        "w_mla_o": nrm(ks[10], (L, MLA_WIDTH, D), MLA_WIDTH ** -0.5),
        "w_out": nrm(ks[11], (L, D, D), D ** -0.5),
        "norm_moe": gain(ks[12], (L, D)),
        "w_router": nrm(ks[13], (L, D, N_EXPERTS), D ** -0.5),
        "moe_w1": nrm(ks[14], (L, N_EXPERTS, D, EXPERT_FF), D ** -0.5),
        "moe_w3": nrm(ks[15], (L, N_EXPERTS, D, EXPERT_FF), D ** -0.5),
        "moe_w2": nrm(ks[16], (L, N_EXPERTS, EXPERT_FF, D), EXPERT_FF ** -0.5),
        "norm_ple": gain(ks[17], (L, D)),
        "ple_gate_w": nrm(ks[18], (L, D, D), D ** -0.5),
        "ple_w": nrm(ks[19], (L, PLE_DIM, D), PLE_DIM ** -0.5),
        "norm_final": gain(ks[20], (D,)),
    }


def reference(x, p, norm_mix, w_in, na_rpb, mla_q_norm, mla_wq_up, mla_kv_norm, mla_wkv_up,
              w_na_o, w_mla_o, w_out, norm_moe, w_router, moe_w1, moe_w3, moe_w2,
              norm_ple, ple_gate_w, ple_w, norm_final):
    B, S, D = x.shape
    t = jnp.arange(S)
    rows_pos = t // GRID_W
    cols_pos = t % GRID_W
    for i in range(DEPTH):
        h = rmsnorm(x, norm_mix[i])
        proj = h @ w_in[i]
        na_q, na_k, na_v, q_lat, kv_lat, k_rope, gate_a, gate_b = jnp.split(proj, IN_CUTS, axis=-1)
        y_a = neighbourhood_attention(na_q.reshape(B, S, NA_HEADS, NA_HEAD_DIM),
                                      na_k.reshape(B, S, NA_HEADS, NA_HEAD_DIM),
                                      na_v.reshape(B, S, NA_HEADS, NA_HEAD_DIM),
                                      na_rpb[i]) @ w_na_o[i]
        y_b = latent_attention(q_lat, kv_lat, k_rope, mla_q_norm[i], mla_wq_up[i],
                               mla_kv_norm[i], mla_wkv_up[i], rows_pos, cols_pos) @ w_mla_o[i]
        merged = jax.nn.sigmoid(gate_a) * y_a + jax.nn.sigmoid(gate_b) * y_b
        x = x + merged @ w_out[i]
        x = x + expert_choice_moe(rmsnorm(x, norm_moe[i]), w_router[i], moe_w1[i], moe_w3[i], moe_w2[i])
        ple_gate = jax.nn.sigmoid(rmsnorm(x, norm_ple[i]) @ ple_gate_w[i])
        x = x + ple_gate * (p[i] @ ple_w[i])
    return rmsnorm(x, norm_final)
```

```python
from contextlib import ExitStack
import numpy as np
import concourse.bass as bass
import concourse.mybir as mybir
from concourse.bass_utils import run_bass_kernel_spmd

F32 = mybir.dt.float32
BF16 = mybir.dt.bfloat16
I32 = mybir.dt.int32
AF = mybir.ActivationFunctionType
ALU = mybir.AluOpType
AX = mybir.AxisListType

D = 1024
NDMA_SEM = 24
EPS = 1e-6
MASKV = -240000.0


class Prog:
    ENGS = ("pe", "dve", "act", "pool", "sp")

    def __init__(self, nc):
        self.nc = nc
        self.stream = {e: [] for e in self.ENGS}
        self.cnt = {e: 0 for e in self.ENGS}
        self.sem = {}
        self.known = {e: {} for e in self.ENGS}
        self.last_w = {}
        self.readers = {}
        self.dq = {}
        self._ctx = []
        for e in ("pe", "dve", "act", "pool"):
            self.sem[e] = self._mksem("c_" + e)
        for q in ("sp", "pool", "act"):
            self.dq[q] = {"sems": [self._mksem(f"d_{q}{i}") for i in range(NDMA_SEM)], "n": 0}
        self.nops = 0

    def _mksem(self, name):
        cm = self.nc.semaphore(name)
        s = cm.__enter__()
        self._ctx.append(cm)
        return s

    def _need(self, eng, ev):
        sem, val, src, is_dma = ev
        if (not is_dma) and src == eng and eng == "pe":
            return
        k = self.known[eng]
        if k.get(id(sem), 0) >= val:
            return
        k[id(sem)] = val
        self.stream[eng].append(("wait", sem, val))

    def _deps(self, eng, reads, writes):
        for key in reads:
            ev = self.last_w.get(key)
            if ev is not None:
                self._need(eng, ev)
        for key in writes:
            ev = self.last_w.get(key)
            if ev is not None:
                self._need(eng, ev)
            for ev in self.readers.get(key, ()):
                self._need(eng, ev)

    def _commit(self, ev, reads, writes):
        for key in reads:
            self.readers.setdefault(key, []).append(ev)
        for key in writes:
            self.last_w[key] = ev
            self.readers[key] = []

    def op(self, eng, fn, reads=(), writes=()):
        self._deps(eng, reads, writes)
        self.cnt[eng] += 1
        ev = (self.sem[eng], self.cnt[eng], eng, False)
        self.stream[eng].append(("op", fn, self.sem[eng], 1))
        self._commit(ev, reads, writes)
        self.nops += 1
        return ev

    def dma(self, q, fn, reads=(), writes=()):
        d = self.dq[q]
        i = d["n"]
        d["n"] += 1
        sem = d["sems"][i % NDMA_SEM]
        rnd = i // NDMA_SEM
        if rnd > 0:
            self._need(q, (sem, 16 * rnd, q, True))
        self._deps(q, reads, writes)
        ev = (sem, 16 * (rnd + 1), q, True)
        self.stream[q].append(("op", fn, sem, 16))
        self._commit(ev, reads, writes)
        self.nops += 1
        return ev

    def barrier(self):
        for e in self.ENGS:
            for e2 in ("pe", "dve", "act", "pool"):
                if self.cnt[e2] > 0:
                    self._need(e, (self.sem[e2], self.cnt[e2], e2, False))
            for q, d in self.dq.items():
                for j, sem in enumerate(d["sems"]):
                    n = (d["n"] - j + NDMA_SEM - 1) // NDMA_SEM if d["n"] > j else 0
                    if n > 0:
                        self._need(e, (sem, 16 * n, q, True))
        self.last_w = {}
        self.readers = {}

    def flush(self):
        self.barrier()
        nc = self.nc
        emap = {"pe": "tensor", "dve": "vector", "act": "scalar", "pool": "gpsimd", "sp": "sync"}
        with nc.Block() as block:
            for e in self.ENGS:
                items = self.stream[e]

                def body(eng, items=items):
                    self.regcache = {}
                    for it in items:
                        if it[0] == "wait":
                            eng.wait_ge(it[1], it[2])
                        else:
                            it[1](eng).then_inc(it[2], it[3])

                getattr(block, emap[e])(body)
        self.stream = {e: [] for e in self.ENGS}

    def reg(self, eng, val):
        if val not in self.regcache:
            self.regcache[val] = eng.to_reg(val)
        return self.regcache[val]

    def mm(self, out, lhsT, rhs, start, stop, reads, writes):
        return self.op("pe", lambda e: e.matmul(out, lhsT=lhsT, rhs=rhs, start=start, stop=stop), reads, writes)

    def tr(self, out, in_, ident, reads, writes):
        return self.op("pe", lambda e: e.transpose(out=out, in_=in_, identity=ident), reads, writes)

    def act(self, out, in_, func, reads, writes, scale=1.0, accum_out=None):
        if accum_out is None:
            return self.op("act", lambda e: e.activation(out=out, in_=in_, func=func, scale=scale), reads, writes)
        return self.op("act", lambda e: e.activation(out=out, in_=in_, func=func, scale=scale, accum_out=accum_out),
                       reads, writes)

    def tt(self, eng, out, in0, in1, op, reads, writes):
        return self.op(eng, lambda e: e.tensor_tensor(out=out, in0=in0, in1=in1, op=op), reads, writes)

    def ts(self, eng, out, in0, s1, s2, op0, op1, reads, writes):
        if op1 is None:
            return self.op(eng, lambda e: e.tensor_scalar(out=out, in0=in0, scalar1=s1, scalar2=None, op0=op0),
                           reads, writes)
        return self.op(eng, lambda e: e.tensor_scalar(out=out, in0=in0, scalar1=s1, scalar2=s2, op0=op0, op1=op1),
                       reads, writes)

    def stt(self, out, in0, scalar, in1, op0, op1, reads, writes):
        return self.op("dve", lambda e: e.scalar_tensor_tensor(out=out, in0=in0, scalar=scalar, in1=in1, op0=op0, op1=op1),
                       reads, writes)

    def cp(self, eng, out, in_, reads, writes):
        if eng == "act":
            return self.op("act", lambda e: e.copy(out=out, in_=in_), reads, writes)
        return self.op(eng, lambda e: e.tensor_copy(out=out, in_=in_), reads, writes)

    def ld(self, q, out, in_, reads, writes):
        return self.dma(q, lambda e: e.dma_start(out=out, in_=in_), reads, writes)


def build(S=4096, L=2, dbg=()):
    NT = S // 128
    NB = S // 512
    ROWS = S // 64
    CAP = 2 * S // 16
    NG = CAP // 128
    NITER = 30
    nc = bass.Bass("TRN2", target_bir_lowering=False)
    P = Prog(nc)

    def din(name, shape, dt=F32):
        return nc.dram_tensor(name, list(shape), dt, kind="ExternalInput").ap()

    def dscr(name, shape, dt):
        return nc.dram_tensor(name, list(shape), dt).ap()

    x_in = din("x", [S, D])
    p_in = din("p", [L, S, 256])
    norm_mix = din("norm_mix", [L, D])
    w_in = din("w_in_ext", [L, D, 4352])
    tz_in = din("tz", [L, 128, 8 * 14 * 64])
    colmask_in = din("colmask", [128, 64])
    rope_in = din("rope", [2, 32, S])
    q_norm = din("mla_q_norm", [L, 384])
    wq_in = din("wq_ext", [L, 384, 8 * 192])
    kv_norm = din("mla_kv_norm", [L, 256])
    wkv_in = din("wkv_re", [L, 256, 1024])
    w_na_o = din("w_na_o", [L, 512, D])
    w_mla_o = din("w_mla_o", [L, 512, D])
    w_out = din("w_out", [L, D, D])
    norm_moe = din("norm_moe", [L, D])
    w_router = din("w_router", [L, D, 16])
    moe_w1 = din("moe_w1", [L, 16, D, D])
    moe_w3 = din("moe_w3", [L, 16, D, D])
    moe_w2 = din("moe_w2", [L, 16, D, D])
    norm_ple = din("norm_ple", [L, D])
    ple_gate_w = din("ple_gate_w", [L, D, D])
    ple_w = din("ple_w", [L, 256, D])
    norm_final = din("norm_final", [1, D])
    y_out = nc.dram_tensor("y", [S, D], F32, kind="ExternalOutput").ap()

    xs = dscr("xs", [S, D], F32)
    qnT = dscr("qnT", [512, S], BF16)
    knT = dscr("knT", [512, S], BF16)
    vn_aug = dscr("vn_aug", [S, 520], BF16)
    qmT = dscr("qmT", [8, 96, S], BF16)
    kmT = dscr("kmT", [8, 64, S], BF16)
    kpeT = dscr("kpeT", [32, S], BF16)
    vm_aug = dscr("vm_aug", [S, 584], BF16)
    gaT = dscr("gaT", [D, S], BF16)
    gbT = dscr("gbT", [D, S], BF16)
    naT = dscr("naT", [512, S], BF16)
    mlaT = dscr("mlaT", [512, S], BF16)
    RW = 1152
    h2rows = dscr("h2rows", [S, RW], BF16)
    xg = [dscr(f"xg{e}", [CAP, RW], BF16) for e in range(16)]
    dbg_t = {}
    for name, shape, dt in dbg:
        dbg_t[name] = nc.dram_tensor("dbg_" + name, list(shape), dt, kind="ExternalOutput").ap()

    def palloc(name, shape, dt):
        return nc.alloc_sbuf_tensor(name, list(shape), dt).ap()

    identf = palloc("identf", [128, 128], F32)
    identb = palloc("identb", [128, 128], BF16)
    onesf = palloc("onesf", [128, 128], F32)
    lstr = palloc("lstr", [128, 128], F32)
    posi = palloc("posi", [128, NT, 16], I32)
    aff = palloc("aff", [128, NT, 16], F32)
    P.op("pool", lambda e: e.memset(identf, 0.0), writes=["identf"])
    P.op("pool", lambda e: e.affine_select(out=identf, in_=identf, pattern=[[-1, 128]], compare_op=ALU.not_equal,
                                           fill=1.0, base=0, channel_multiplier=1), ["identf"], ["identf"])
    P.cp("dve", identb, identf, ["identf"], ["identb"])
    P.op("pool", lambda e: e.memset(onesf, 1.0), writes=["onesf"])
    P.op("pool", lambda e: e.memset(lstr, 1.0), writes=["lstr"])
    P.op("pool", lambda e: e.affine_select(out=lstr, in_=lstr, pattern=[[1, 128]], compare_op=ALU.is_gt,
                                           fill=0.0, base=0, channel_multiplier=-1), ["lstr"], ["lstr"])
    P.flush()

    def blkview(ap2d, blk):
        return ap2d.rearrange("(c p) s -> p c s", p=128)[:, :, blk * 512:(blk + 1) * 512]

    def rows_view(ap2d, r0, nt):
        return ap2d[r0:r0 + 128 * nt, :].rearrange("(t p) f -> p t f", p=128)

    hT_d = dscr("hT_d", [D, S], BF16)

    for l in range(L):
        xsrc = x_in if l == 0 else xs

        with ExitStack() as es:
            def sb(name, shape, dt):
                return es.enter_context(nc.sbuf_tensor(f"p1_{l}_{name}", list(shape), dt)).ap()

            def psb(name, shape=(128, 512), dt=F32):
                return es.enter_context(nc.psum_tensor(f"p1_{l}_{name}", list(shape), dt)).ap()

            win = sb("win", [128, 8, 2304], BF16)
            wq = sb("wq", [128, 3, 1536], BF16)
            wkv = sb("wkv", [128, 2, 1024], BF16)
            gmix = sb("gmix", [128, D], F32)
            gq = sb("gq", [128, 3], F32)
            gkv = sb("gkv", [128, 2], F32)
            xb2 = [sb(f"xblk{i}", [128, 4, D], F32) for i in range(2)]
            junk = sb("junk", [128, D], BF16)
            ss2 = [sb(f"ss{i}", [128, 4], F32) for i in range(2)]
            rstd2 = [sb(f"rstd{i}", [128, 4], F32) for i in range(2)]
            h2b = [sb(f"h{i}", [128, 4, D], BF16) for i in range(2)]
            hT = sb("hT", [128, 8, 512], BF16)
            stq = sb("stq", [128, 4, 512], BF16)
            stk = sb("stk", [128, 4, 512], BF16)
            stv = sb("stv", [128, 4, 520], BF16)
            stv2 = sb("stv2", [128, 4, 584], BF16)
            qlat = sb("qlat", [128, 3, 512], F32)
            kvlat = sb("kvlat", [128, 2, 512], F32)
            sq = sb("sq", [128, 3, 512], F32)
            rt = sb("rt", [128, 512], F32)
            qn_ = sb("qn_", [128, 3, 512], BF16)
            kvn_ = sb("kvn_", [128, 2, 512], BF16)
            cs2 = [sb(f"cs{i}", [128, 2, 512], F32) for i in range(2)]
            t1 = sb("t1", [128, 512], F32)
            t2 = sb("t2", [128, 512], F32)
            kpest = sb("kpest", [128, 512], BF16)
            qst = sb("qst", [128, 8, 512], BF16)
            kst = sb("kst", [128, 8, 512], BF16)
            ps = [psb(f"ps{i}") for i in range(6)]
            pst = [psb(f"pst{i}", (128, 1024), BF16) for i in range(2)]
            psi = [0]

            def nps():
                i = psi[0] % 6
                psi[0] += 1
                return ps[i], ("ps", i)

            for c in range(8):
                rs = slice(c * 128, (c + 1) * 128)
                P.ld("pool", win[:, c, 0:1024], w_in[l, rs, 0:1024], [], [("win", c, 0)])
                P.ld("pool", win[:, c, 1024:2048], w_in[l, rs, 1024:2048], [], [("win", c, 1)])
                P.ld("pool", win[:, c, 2048:2208], w_in[l, rs, 2048:2208], [], [("win", c, 2)])
                P.ld("pool", win[:, c, 2208:2304], w_in[l, rs, 4256:4352], [], [("win", c, 3)])
            for c in range(3):
                for j in range(2):
                    P.ld("pool", wq[:, c, j * 768:(j + 1) * 768], wq_in[l, c * 128:(c + 1) * 128, j * 768:(j + 1) * 768],
                         [], [("wq", c)] if j == 1 else [("wq0", c)])
            for c in range(2):
                P.ld("pool", wkv[:, c, :], wkv_in[l, c * 128:(c + 1) * 128, :], [], [("wkv", c)])
            P.ld("sp", gmix, norm_mix[l].partition_broadcast(128), [], ["gmix"])
            for c in range(3):
                P.ld("sp", gq[:, c:c + 1], q_norm[l, c * 128:(c + 1) * 128].rearrange("(p o) -> p o", o=1), [], ["gq"])
            for c in range(2):
                P.ld("sp", gkv[:, c:c + 1], kv_norm[l, c * 128:(c + 1) * 128].rearrange("(p o) -> p o", o=1), [], ["gkv"])
            P.op("pool", lambda e: e.memset(stv, 1.0), writes=["stv"])
            P.op("pool", lambda e: e.memset(stv2, 1.0), writes=["stv2"])

            def wkeys(c, col0, m):
                f = lambda col: 3 if col >= 2208 else col // 1024
                return list({("win", c, f(col0)), ("win", c, f(col0 + m - 1))})

            def pro_a(blk):
                i = blk % 2
                tok0 = blk * 512
                xblk, cs, ss, rstd, h = xb2[i], cs2[i], ss2[i], rstd2[i], h2b[i]
                P.ld("sp", xblk, rows_view(xsrc, tok0, 4), [], [("xblk", i)])
                P.ld("sp", cs[64:96, 0, :], rope_in[0, :, tok0:tok0 + 512], [], [("cs", i)])
                P.ld("sp", cs[64:96, 1, :], rope_in[1, :, tok0:tok0 + 512], [], [("cs", i)])
                for t in range(4):
                    P.act(junk, xblk[:, t, :], AF.Square, [("xblk", i)], ["junk", ("ss", i)], accum_out=ss[:, t:t + 1])
                P.ts("dve", rstd, ss, 1.0 / D, EPS, ALU.mult, ALU.add, [("ss", i)], [("rstd", i)])
                P.op("act", lambda e: e.sqrt(out=rstd, in_=rstd), [("rstd", i)], [("rstd", i)])
                P.op("dve", lambda e: e.reciprocal(out=rstd, in_=rstd), [("rstd", i)], [("rstd", i)])
                for t in range(4):
                    P.stt(h[:, t, :], xblk[:, t, :], rstd[:, t:t + 1], gmix, ALU.mult, ALU.mult,
                          [("xblk", i), ("rstd", i), "gmix"], [("h", i, t)])

            def pro_b(blk):
                i = blk % 2
                h = h2b[i]
                for t in range(4):
                    pt = pst[t % 2]
                    for c in range(8):
                        P.tr(pt[:, c * 128:(c + 1) * 128], h[:, t, c * 128:(c + 1) * 128], identb,
                             [("h", i, t), "identb"], [("pst", t % 2)])
                    P.cp("act" if t % 2 else "dve", hT[:, :, t * 128:(t + 1) * 128],
                         pt.rearrange("p (c n) -> p c n", c=8), [("pst", t % 2)], ["hT"])
                P.ld("sp", blkview(hT_d, blk), hT, ["hT"], ["hT_d"])

            pro_a(0)
            pro_b(0)
            for blk in range(NB):
                tok0 = blk * 512
                cs = cs2[blk % 2]
                csk = ("cs", blk % 2)
                if blk + 1 < NB:
                    pro_a(blk + 1)

                def fm_group(col0, m):
                    pp, pk = nps()
                    for c in range(8):
                        P.mm(pp[0:m, :], win[:, c, col0:col0 + m], hT[:, c, :], c == 0, c == 7,
                             ["hT"] + wkeys(c, col0, m), [pk])
                    return pp, pk

                def lat_sq(src, nch, tag):
                    for c in range(nch):
                        P.act(sq[:, c, :], src[:, c, :], AF.Square, [tag], ["sq"])

                def lat_fin(src, nch, dim, gvec, gkey, dst, tag):
                    pp, pk = nps()
                    for c in range(nch):
                        P.mm(pp, onesf, sq[:, c, :], c == 0, c == nch - 1, ["sq", "onesf"], [pk])
                    P.ts("dve", rt, pp, 1.0 / dim, EPS, ALU.mult, ALU.add, [pk], ["rt"])
                    P.op("act", lambda e: e.sqrt(out=rt, in_=rt), ["rt"], ["rt"])
                    P.op("dve", lambda e: e.reciprocal(out=rt, in_=rt), ["rt"], ["rt"])
                    for c in range(nch):
                        P.stt(dst[:, c, :], src[:, c, :], gvec[:, c:c + 1], rt, ALU.mult, ALU.mult,
                              [tag, "rt", gkey], [tag + "n"])

                for j in range(3):
                    pp, pk = fm_group(1536 + j * 128, 128)
                    P.cp("act", qlat[:, j, :], pp, [pk], ["qlat"])
                for j in range(2):
                    pp, pk = fm_group(1920 + j * 128, 128)
                    P.cp("dve", kvlat[:, j, :], pp, [pk], ["kvlat"])
                lat_sq(qlat, 3, "qlat")
                for j in range(4):
                    pp, pk = fm_group(j * 128, 128)
                    P.cp("act", stq[:, j, :], pp, [pk], ["stq"])
                P.ld("sp", blkview(qnT, blk), stq, ["stq"], ["qnT"])
                lat_fin(qlat, 3, 384, gq, "gq", qn_, "qlat")
                lat_sq(kvlat, 2, "kvlat")
                for j in range(4):
                    pp, pk = fm_group(512 + j * 128, 128)
                    P.cp("dve", stk[:, j, :], pp, [pk], ["stk"])
                P.ld("sp", blkview(knT, blk), stk, ["stk"], ["knT"])
                lat_fin(kvlat, 2, 256, gkv, "gkv", kvn_, "kvlat")
                for t in range(4):
                    pp, pk = nps()
                    for c in range(8):
                        P.mm(pp, hT[:, c, t * 128:(t + 1) * 128], win[:, c, 1024:1536], c == 0, c == 7,
                             ["hT", ("win", c, 1)], [pk])
                    P.cp("act", stv[:, t, :].rearrange("p (h d) -> p h d", d=65)[:, :, 0:64],
                         pp.rearrange("p (h d) -> p h d", d=64), [pk], ["stv"])
                P.ld("sp", rows_view(vn_aug, tok0, 4), stv, ["stv"], ["vn_aug"])
                ppa, pka = fm_group(2112, 96)
                ppb, pkb = fm_group(2208, 96)
                P.tt("dve", t1[64:96, :], ppa[64:96, :], cs[64:96, 0, :], ALU.mult, [pka, csk], ["t1"])
                P.tt("dve", t2[64:96, :], ppb[64:96, :], cs[64:96, 1, :], ALU.mult, [pkb, csk], ["t2"])
                P.tt("pool", kpest[64:96, :], t1[64:96, :], t2[64:96, :], ALU.add, ["t1", "t2"], ["kpest"])
                P.ld("sp", kpeT[:, tok0:tok0 + 512], kpest[64:96, :], ["kpest"], ["kpeT"])
                if blk + 1 < NB:
                    pro_b(blk + 1)

                for hh in range(8):
                    ppa, pka = nps()
                    ppb, pkb = nps()
                    for c in range(3):
                        P.mm(ppa[0:96, :], wq[:, c, hh * 192:hh * 192 + 96], qn_[:, c, :], c == 0, c == 2,
                             ["qlatn", ("wq", c), ("wq0", c)], [pka])
                    for c in range(3):
                        P.mm(ppb[0:96, :], wq[:, c, hh * 192 + 96:hh * 192 + 192], qn_[:, c, :], c == 0, c == 2,
                             ["qlatn", ("wq", c), ("wq0", c)], [pkb])
                    P.cp("act", qst[0:64, hh, :], ppa[0:64, :], [pka], ["qst"])
                    P.tt("dve", t1[64:96, :], ppa[64:96, :], cs[64:96, 0, :], ALU.mult, [pka, csk], ["t1"])
                    P.tt("dve", t2[64:96, :], ppb[64:96, :], cs[64:96, 1, :], ALU.mult, [pkb, csk], ["t2"])
                    P.tt("pool", qst[64:96, hh, :], t1[64:96, :], t2[64:96, :], ALU.add, ["t1", "t2"], ["qst"])
                P.ld("sp", qmT.rearrange("h r s -> r h s")[:, :, tok0:tok0 + 512], qst[0:96], ["qst"], ["qmT"])
                for hh in range(8):
                    pp, pk = nps()
                    for c in range(2):
                        P.mm(pp[0:64, :], wkv[:, c, hh * 64:(hh + 1) * 64], kvn_[:, c, :], c == 0, c == 1,
                             ["kvlatn", ("wkv", c)], [pk])
                    P.cp("act" if hh % 2 else "dve", kst[0:64, hh, :], pp[0:64, :], [pk], ["kst"])
                P.ld("sp", kmT.rearrange("h r s -> r h s")[:, :, tok0:tok0 + 512], kst[0:64], ["kst"], ["kmT"])
                for t in range(4):
                    pp, pk = nps()
                    for c in range(2):
                        P.mm(pp, kvn_[:, c, t * 128:(t + 1) * 128], wkv[:, c, 512:1024], c == 0, c == 1,
                             ["kvlatn", ("wkv", c)], [pk])
                    P.cp("act", stv2[:, t, 0:520].rearrange("p (h d) -> p h d", d=65)[:, :, 0:64],
                         pp.rearrange("p (h d) -> p h d", d=64), [pk], ["stv2"])
                P.ld("sp", rows_view(vm_aug, tok0, 4), stv2, ["stv2"], ["vm_aug"])
            P.flush()

        with ExitStack() as es:
            def sb(name, shape, dt):
                return es.enter_context(nc.sbuf_tensor(f"p1b_{l}_{name}", list(shape), dt)).ap()

            def psb(name, shape=(128, 512), dt=F32):
                return es.enter_context(nc.psum_tensor(f"p1b_{l}_{name}", list(shape), dt)).ap()

            wing = sb("wing", [128, 8, 2048], BF16)
            hTb = [sb(f"hTb{i}", [128, 8, 512], BF16) for i in range(2)]
            stg = [sb(f"stg{i}", [128, 8, 512], BF16) for i in range(2)]
            ps = [psb(f"ps{i}") for i in range(8)]
            for c in range(8):
                for j in range(2):
                    P.ld("pool", wing[:, c, j * 1024:(j + 1) * 1024],
                         w_in[l, c * 128:(c + 1) * 128, 2208 + j * 1024:2208 + (j + 1) * 1024], [], [("wing", c, j)])
            gi_ = 0
            P.ld("sp", hTb[0], blkview(hT_d, 0), [], [("hTb", 0)])
            for blk in range(NB):
                hb = hTb[blk % 2]
                if blk + 1 < NB:
                    P.ld("sp", hTb[(blk + 1) % 2], blkview(hT_d, blk + 1), [], [("hTb", (blk + 1) % 2)])
                for gi, gdst in enumerate((gaT, gbT)):
                    sg_ = stg[gi]
                    for j in range(8):
                        pp, pk = ps[gi_ % 8], ("ps", gi_ % 8)
                        gi_ += 1
                        col0 = gi * 1024 + j * 128
                        for c in range(8):
                            P.mm(pp, wing[:, c, col0:col0 + 128], hb[:, c, :], c == 0, c == 7,
                                 [("hTb", blk % 2), ("wing", c, gi)], [pk])
                        P.act(sg_[:, j, :], pp, AF.Sigmoid, [pk], [("stg", gi)])
                    P.ld("sp", blkview(gdst, blk), sg_, [("stg", gi)], [("gT", gi)])
            P.flush()

        with ExitStack() as es:
            def sb(name, shape, dt):
                return es.enter_context(nc.sbuf_tensor(f"p2_{l}_{name}", list(shape), dt)).ap()

            def psb(name, shape=(128, 512), dt=F32):
                return es.enter_context(nc.psum_tensor(f"p2_{l}_{name}", list(shape), dt)).ap()

            qn = sb("qn", [128, 4, S], BF16)
            kn = sb("kn", [128, 4, S], BF16)
            vE = sb("vE", [128, NT, 520], BF16)
            vO = sb("vO", [128, NT, 520], BF16)
            tz = sb("tz", [128, 112, 64], F32)
            cmask = sb("cmask", [128, 64], F32)
            tb = sb("tb", [128, 8, 14, 64], BF16)
            P.ld("sp", tz, tz_in[l].rearrange("p (a q) -> p a q", q=64), [], ["tz"])
            P.ld("sp", cmask, colmask_in, [], ["cmask"])
            P.stt(tb.rearrange("p h d q -> p (h d) q"), tz, 8.0, cmask.unsqueeze(1).broadcast_to([128, 112, 64]),
                  ALU.mult, ALU.add, ["tz", "cmask"], ["tb"])
            pT = [sb(f"pT{i}", [128, 512], BF16) for i in range(2)]
            rec = sb("rec", [64, 8], F32)
            otok = sb("otok", [64, 8, 64], BF16)
            nast = sb("nast", [128, 4, 512], BF16)
            pss = [psb(f"pss{i}") for i in range(3)]
            pso = [psb(f"pso{i}") for i in range(2)]
            ptr = [psb(f"ptr{i}", (128, 1024), BF16) for i in range(2)]

            P.ld("sp", qn, qnT.rearrange("(c p) s -> p c s", p=128), [], ["qn"])
            P.ld("sp", kn, knT.rearrange("(c p) s -> p c s", p=128), [], ["kn"])
            P.ld("sp", vE, rows_view(vn_aug, 0, NT), [], ["vE"])
            P.ld("sp", vO[:, 0:NT - 1, :], rows_view(vn_aug, 64, NT - 1), [], ["vO"])
            items = [(r, hp) for r in range(ROWS) for hp in range(4)]
            qpad = [sb(f"qpad{i}", [128, 4, 128], BF16) for i in range(2)]
            for i_ in range(2):
                P.op("pool", lambda e, i_=i_: e.memset(qpad[i_], 0.0), [], [("qpad", i_)])

            def na_qpad(r):
                qp = qpad[r % 2]
                P.cp("pool", qp[0:64, :, 0:64], qn[0:64, :, 64 * r:64 * r + 64], ["qn"], [("qpad", r % 2)])
                P.cp("pool", qp[64:128, :, 64:128], qn[64:128, :, 64 * r:64 * r + 64], ["qn"], [("qpad", r % 2)])

            def na_qk(idx):
                r, hp = items[idx]
                r0 = min(max(r - 4, 0), ROWS - 8)
                kb = 64 * r0
                psS = pss[idx % 3]
                d0 = r0 - r + 7
                P.mm(psS, identb, tb[:, 2 * hp:2 * hp + 2, d0:d0 + 7:2, :].rearrange("p h c q -> p c h q"), True, False,
                     ["tb", "identb"], [("pss", idx % 3)])
                for c in range(4):
                    P.mm(psS[:, c * 128:(c + 1) * 128], kn[:, hp, kb + 128 * c:kb + 128 * c + 128],
                         qpad[r % 2][:, hp, :], False, c == 3, ["kn", ("qpad", r % 2)], [("pss", idx % 3)])
                P.act(pT[idx % 2], psS, AF.Exp, [("pss", idx % 3)], [("pT", idx % 2)], scale=0.125)

            def na_pv(idx):
                r, hp = items[idx]
                r0 = min(max(r - 4, 0), ROWS - 8)
                kb = 64 * r0
                pTb = pT[idx % 2]
                for h2 in range(2):
                    hh = 2 * hp + h2
                    po = pso[hh // 4]
                    for c in range(4):
                        if r0 % 2 == 0:
                            vch = vE[:, kb // 128 + c, hh * 65:(hh + 1) * 65]
                        else:
                            vch = vO[:, (kb - 64) // 128 + c, hh * 65:(hh + 1) * 65]
                        P.mm(po[0:64, (hh % 4) * 65:(hh % 4) * 65 + 65], pTb[:, c * 128 + h2 * 64:c * 128 + h2 * 64 + 64], vch,
                             c == 0, c == 3, [("pT", idx % 2), "vE", "vO"], [("pso", hh // 4)])

            na_qpad(0)
            na_qk(0)
            for r in range(ROWS):
                if r + 1 < ROWS:
                    na_qpad(r + 1)
                for hp in range(4):
                    idx = r * 4 + hp
                    if idx + 1 < len(items):
                        na_qk(idx + 1)
                    na_pv(idx)
                for g in range(2):
                    pov = pso[g][0:64, 0:260].rearrange("p (h d) -> p h d", d=65)
                    P.op("dve", lambda e, pov=pov, g=g: e.reciprocal(out=rec[:, g * 4:(g + 1) * 4].unsqueeze(2),
                                                                    in_=pov[:, :, 64:65]),
                         [("pso", g)], ["rec"])
                    P.tt("dve", otok[:, g * 4:(g + 1) * 4, :], pov[:, :, 0:64],
                         rec[:, g * 4:(g + 1) * 4].unsqueeze(2).broadcast_to([64, 4, 64]), ALU.mult,
                         [("pso", g), "rec"], ["otok"])
                ptb = ptr[r % 2]
                of = otok.rearrange("p h d -> p (h d)")
                for c in range(4):
                    P.tr(ptb[:, c * 64:(c + 1) * 64], of[:, c * 128:(c + 1) * 128], identb[0:64, 0:64],
                         ["otok", "identb"], [("ptr", r % 2)])
                rr = r % 8
                P.cp("act", nast[:, :, rr * 64:(rr + 1) * 64], ptb[:, 0:256].rearrange("p (c q) -> p c q", c=4),
                     [("ptr", r % 2)], ["nast"])
                if rr == 7:
                    P.ld("sp", blkview(naT, r // 8), nast, ["nast"], ["naT"])
            P.flush()

        with ExitStack() as es:
            def sb(name, shape, dt):
                return es.enter_context(nc.sbuf_tensor(f"p3_{l}_{name}", list(shape), dt)).ap()

            def psb(name, shape=(128, 512), dt=F32):
                return es.enter_context(nc.psum_tensor(f"p3_{l}_{name}", list(shape), dt)).ap()

            vm = sb("vm", [128, NT, 584], BF16)
            kT = [sb(f"kT{i}", [128, S], BF16) for i in range(2)]
            qT = [sb(f"qT{i}", [128, S], BF16) for i in range(2)]
            for i_ in range(2):
                P.op("pool", lambda e, i_=i_: e.memset(kT[i_][96:128, :], 0.0), [], [("kT", i_)])
                P.op("pool", lambda e, i_=i_: e.memset(qT[i_][96:128, :], 0.0), [], [("qT", i_)])
            pTm = [sb(f"pTm{i}", [128, 1024], BF16) for i in range(3)]
            rs = [sb(f"rs{i}", [128, 512], F32) for i in range(2)]
            ov = [sb(f"ov{i}", [64, 512], F32) for i in range(2)]
            mst = [sb(f"mst{i}", [64, 512], BF16) for i in range(2)]
            pss = [psb(f"pss{i}", (128, 1024)) for i in range(3)]
            pso = [psb(f"pso{i}") for i in range(1)]
            pbc = [psb(f"pbc{i}") for i in range(1)]
            P.ld("sp", vm, rows_view(vm_aug, 0, NT), [], ["vm"])
            sc = 96.0 ** -0.5
            it = 0
            qi = 0
            pending = []

            def fin1(a, hh, qb):
                P.cp("dve", rs[a][64:65, :], pso[0][64:65, :], [("pso", 0)], [("rs", a)])
                P.cp("dve", ov[a], pso[0][0:64, :], [("pso", 0)], [("ov", a)])
                P.op("dve", lambda e: e.reciprocal(out=rs[a][64:65, :], in_=rs[a][64:65, :]), [("rs", a)], [("rs", a)])

            def fin2(a, hh, qb):
                P.mm(pbc[0][0:64, :], onesf[64:65, 0:64], rs[a][64:65, :], True, True, [("rs", a), "onesf"], [("pbc", 0)])
                P.tt("dve", mst[a], ov[a], pbc[0][0:64, :], ALU.mult, [("ov", a), ("pbc", 0)], [("mst", a)])
                P.ld("sp", mlaT[hh * 64:(hh + 1) * 64, qb * 512:(qb + 1) * 512], mst[a], [("mst", a)], ["mlaT"])

            for hh in range(8):
                b = hh % 2
                P.ld("sp", kT[b][0:64, :], kmT[hh], [], [("kT", b)])
                P.ld("sp", kT[b][64:96, :], kpeT, [], [("kT", b)])
                P.ld("sp", qT[b][0:96, :], qmT[hh], [], [("qT", b)])
                for qb in range(NB):
                    a = qi % 2

                    NP2 = NT // 2

                    def qk(m, it0):
                        i2 = (it0 + m) % 3
                        for u in range(2):
                            kc = 2 * m + u
                            P.mm(pss[i2][:, u * 512:(u + 1) * 512], kT[b][:, kc * 128:(kc + 1) * 128],
                                 qT[b][:, qb * 512:(qb + 1) * 512], True, True, [("kT", b), ("qT", b)], [("pss", i2)])
                        P.act(pTm[i2], pss[i2], AF.Exp, [("pss", i2)], [("pTm", i2)], scale=sc)

                    def pv(m, it0):
                        i2 = (it0 + m) % 3
                        for u in range(2):
                            kc = 2 * m + u
                            P.mm(pso[0], vm[:, kc, hh * 65:hh * 65 + 128], pTm[i2][:, u * 512:(u + 1) * 512],
                                 kc == 0, kc == NT - 1, [("pTm", i2), "vm"], [("pso", 0)])

                    qk(0, it)
                    if NP2 > 1:
                        qk(1, it)
                    for m in range(NP2):
                        if m + 2 < NP2:
                            qk(m + 2, it)
                        pv(m, it)
                        if m == min(2, NP2 - 1) and pending:
                            fin2(*pending.pop())
                    it += NP2
                    fin1(a, hh, qb)
                    pending.append((a, hh, qb))
                    qi += 1
            while pending:
                fin2(*pending.pop())
            P.flush()

        with ExitStack() as es:
            def sb(name, shape, dt):
                return es.enter_context(nc.sbuf_tensor(f"p4_{l}_{name}", list(shape), dt)).ap()

            def psb(name, shape=(128, 512), dt=F32):
                return es.enter_context(nc.psum_tensor(f"p4_{l}_{name}", list(shape), dt)).ap()

            wna = sb("wna", [128, 4, D], BF16)
            wml = sb("wml", [128, 4, D], BF16)
            wo = sb("wo", [128, 8, D], BF16)
            wr = sb("wr", [128, 8, 16], F32)
            gmoe = sb("gmoe", [128, D], F32)
            nab2 = [sb(f"nab{i}", [128, 4, 512], BF16) for i in range(2)]
            mlb2 = [sb(f"mlb{i}", [128, 4, 512], BF16) for i in range(2)]
            gab2 = [sb(f"gab{i}", [128, 8, 512], BF16) for i in range(2)]
            gbb2 = [sb(f"gbb{i}", [128, 8, 512], BF16) for i in range(2)]
            ta = sb("ta", [128, 512], F32)
            tb2 = sb("tb2", [128, 512], F32)
            ta1 = sb("ta1", [128, 512], F32)
            tb21 = sb("tb21", [128, 512], F32)
            mg = sb("mg", [128, 8, 512], BF16)
            xb2 = [sb(f"xblk{i}", [128, 4, D], F32) for i in range(2)]
            junk = sb("junk", [128, D], BF16)
            ss = sb("ss", [128, 4], F32)
            rstd = sb("rstd", [128, 4], F32)
            h2f = sb("h2f", [128, D], F32)
            h2T = sb("h2T", [128, 8, 128], F32)
            rows = sb("rows", [128, 4, RW], BF16)
            lg = sb("lg", [128, 16], F32)
            mx = sb("mx", [128, 1], F32)
            sm = sb("sm", [128, 1], F32)
            ps = [psb(f"ps{i}") for i in range(6)]
            ptf = [psb(f"ptf{i}") for i in range(2)]
            psi = [0]

            def nps():
                i = psi[0] % 6
                psi[0] += 1
                return ps[i], ("ps", i)

            for c in range(4):
                P.ld("pool", wna[:, c, :], w_na_o[l, c * 128:(c + 1) * 128, :], [], ["wna"] if c == 3 else [("wna", c)])
                P.ld("pool", wml[:, c, :], w_mla_o[l, c * 128:(c + 1) * 128, :], [], ["wml"] if c == 3 else [("wml", c)])
            for c in range(8):
                P.ld("pool", wo[:, c, :], w_out[l, c * 128:(c + 1) * 128, :], [], [("wo", c)])
            P.ld("sp", wr, w_router[l].rearrange("(c p) e -> p c e", p=128), [], ["wr"])
            P.ld("sp", gmoe, norm_moe[l].partition_broadcast(128), [], ["gmoe"])
            wna_k = ["wna", ("wna", 0), ("wna", 1), ("wna", 2)]
            wml_k = ["wml", ("wml", 0), ("wml", 1), ("wml", 2)]
            xb3 = [xb2[0], xb2[1], sb("xblk2", [128, 4, D], F32)]
            rows2 = [rows, sb("rows1", [128, 4, RW], BF16)]
            for j_ in range(2):
                P.op("pool", lambda e, j_=j_: e.memset(rows2[j_][:, :, D + 34:RW], 0.0), [],
                     [("rows", j_, t) for t in range(4)])
            h2f2 = [h2f, sb("h2f1", [128, D], F32)]
            ss2 = [ss, sb("ss1", [128, 4], F32)]
            rstd2 = [rstd, sb("rstd1", [128, 4], F32)]

            def p4_load(blk):
                i = blk % 2
                P.ld("sp", nab2[i], blkview(naT, blk), [], [("nab", i)])
                P.ld("sp", mlb2[i], blkview(mlaT, blk), [], [("mlb", i)])
                P.ld("sp", gab2[i], blkview(gaT, blk), [], [("gab", i)])
                P.ld("sp", gbb2[i], blkview(gbT, blk), [], [("gbb", i)])
                P.ld("sp", xb3[blk % 3], rows_view(xsrc, blk * 512, 4), [], [("xblk", blk % 3)])

            def router_pre(blk):
                xblk, xk = xb3[blk % 3], ("xblk", blk % 3)
                ssb, rsb, j = ss2[blk % 2], rstd2[blk % 2], blk % 2
                for t in range(4):
                    P.act(junk, xblk[:, t, :], AF.Square, [xk], ["junk", ("ss", j)], accum_out=ssb[:, t:t + 1])
                P.ts("dve", rsb, ssb, 1.0 / D, EPS, ALU.mult, ALU.add, [("ss", j)], [("rstd", j)])
                P.op("act", lambda e: e.sqrt(out=rsb, in_=rsb), [("rstd", j)], [("rstd", j)])
                P.op("dve", lambda e: e.reciprocal(out=rsb, in_=rsb), [("rstd", j)], [("rstd", j)])

            def router_tile(blk, t):
                xblk, xk = xb3[blk % 3], ("xblk", blk % 3)
                rsb, j = rstd2[blk % 2], blk % 2
                rws = rows2[j]
                hf_, hk = h2f2[t % 2], ("h2f", t % 2)
                gt = blk * 4 + t
                P.stt(hf_, xblk[:, t, :], rsb[:, t:t + 1], gmoe, ALU.mult, ALU.mult, [xk, ("rstd", j), "gmoe"], [hk])
                P.cp("pool", rws[:, t, 0:D], hf_, [hk], [("rows", j, t)])
                P.op("pool", lambda e: e.iota(rws[:, t, D:D + 2].bitcast(I32), pattern=[[0, 1]],
                                              base=gt * 128, channel_multiplier=1), [], [("rows", j, t)])
                for c in range(8):
                    pf = ptf[c // 4]
                    P.tr(pf[:, (c % 4) * 128:(c % 4 + 1) * 128], hf_[:, c * 128:(c + 1) * 128], identf,
                         [hk, "identf"], [("ptf", c // 4)])
                P.cp("act", h2T[:, 0:4, :], ptf[0].rearrange("p (c n) -> p c n", c=4), [("ptf", 0)], ["h2T"])
                P.cp("dve", h2T[:, 4:8, :], ptf[1].rearrange("p (c n) -> p c n", c=4), [("ptf", 1)], ["h2T"])
                pp, pk = nps()
                for c in range(8):
                    P.mm(pp[:, 0:16], h2T[:, c, :], wr[:, c, :], c == 0, c == 7, ["h2T", "wr"], [pk])
                P.op("dve", lambda e: e.tensor_reduce(out=mx, in_=pp[:, 0:16], axis=AX.X, op=ALU.max, negate=True),
                     [pk], ["mx"])
                P.op("act", lambda e: e.activation(out=lg, in_=pp[:, 0:16], func=AF.Exp, bias=mx, scale=1.0,
                                                   accum_out=sm), [pk, "mx"], ["lg", "sm"])
                P.op("dve", lambda e: e.reciprocal(out=sm, in_=sm), ["sm"], ["sm"])
                P.ts("dve", aff[:, gt, :], lg, sm, None, ALU.mult, None, ["lg", "sm"], ["aff"])
                P.cp("pool", rws[:, t, D + 2:D + 34].bitcast(F32), aff[:, gt, :], ["aff"], [("rows", j, t)])

            def router_post(blk):
                j = blk % 2
                P.ld("sp", rows_view(h2rows, blk * 512, 4), rows2[j], [("rows", j, t) for t in range(4)], ["h2rows"])

            p4_load(0)
            for blk in range(NB):
                tok0 = blk * 512
                bi = blk % 2
                nab, mlb, gab, gbb, xblk = nab2[bi], mlb2[bi], gab2[bi], gbb2[bi], xb3[blk % 3]
                xk = ("xblk", blk % 3)
                if blk + 1 < NB:
                    p4_load(blk + 1)
                for oc in range(8):
                    pa, pka = nps()
                    pb_, pkb = nps()
                    for c in range(4):
                        P.mm(pa, wna[:, c, oc * 128:(oc + 1) * 128], nab[:, c, :], c == 0, c == 3, [("nab", bi)] + wna_k, [pka])
                    for c in range(4):
                        P.mm(pb_, wml[:, c, oc * 128:(oc + 1) * 128], mlb[:, c, :], c == 0, c == 3, [("mlb", bi)] + wml_k, [pkb])
                    ta_, tb_ = (ta, tb2) if oc % 2 == 0 else (ta1, tb21)
                    P.tt("dve", ta_, pa, gab[:, oc, :], ALU.mult, [pka, ("gab", bi)], [("ta", oc % 2)])
                    P.tt("dve", tb_, pb_, gbb[:, oc, :], ALU.mult, [pkb, ("gbb", bi)], [("tb2", oc % 2)])
                    P.tt("pool", mg[:, oc, :], ta_, tb_, ALU.add, [("ta", oc % 2), ("tb2", oc % 2)], [("mg", oc)])
                    if blk > 0 and oc % 2 == 1:
                        router_tile(blk - 1, oc // 2)
                if blk > 0:
                    router_post(blk - 1)
                for t in range(4):
                    for hf in range(2):
                        pp, pk = nps()
                        for c in range(8):
                            P.mm(pp, mg[:, c, t * 128:(t + 1) * 128], wo[:, c, hf * 512:(hf + 1) * 512], c == 0, c == 7,
                                 [("mg", c), ("wo", c)], [pk])
                        P.tt("dve", xblk[:, t, hf * 512:(hf + 1) * 512], pp, xblk[:, t, hf * 512:(hf + 1) * 512], ALU.add,
                             [pk, xk], [xk])
                P.ld("sp", rows_view(xs, tok0, 4), xblk, [xk], ["xs"])
                router_pre(blk)
            for t in range(4):
                router_tile(NB - 1, t)
            router_post(NB - 1)

            P.flush()

        with ExitStack() as es:
            def sb(name, shape, dt):
                return es.enter_context(nc.sbuf_tensor(f"p5_{l}_{name}", list(shape), dt)).ap()

            def psb(name, shape=(128, 512), dt=F32):
                return es.enter_context(nc.psum_tensor(f"p5_{l}_{name}", list(shape), dt)).ap()

            w1 = [sb(f"w1_{i}", [128, 8, D], BF16) for i in range(3)]
            w3 = [sb(f"w3_{i}", [128, 8, D], BF16) for i in range(3)]
            w2 = [sb(f"w2_{i}", [128, 8, D], BF16) for i in range(2)]
            xgs2 = [sb(f"xgs{i}", [128, NG, RW], BF16) for i in range(2)]
            xgT = sb("xgT", [128, 8, CAP], BF16)
            s1 = sb("s1", [128, CAP], F32)
            aT = sb("aT", [128, 8, CAP], BF16)
            ysb = sb("ysb", [128, NG, D], F32)
            vals2 = [sb(f"vals{i}", [128, NG], F32) for i in range(2)]
            ids2 = [sb(f"ids{i}", [128, NG], I32) for i in range(2)]
            idf = sb("idf", [128, NG], F32)
            lst = sb("lst", [128, NG, 5], F32)
            iof = sb("iof", [128, CAP], F32)
            ioi = sb("ioi", [128, CAP], I32)
            oh = [sb(f"oh{i}", [128, CAP], BF16) for i in range(2)]
            Rt = sb("Rt", [128, NT, 16, 5], BF16)
            tpi = sb("tpi", [128, NT, 2], I32)
            ahi = sb("ahi", [128, NT, 16], BF16)
            amid = sb("amid", [128, NT, 16], BF16)
            res1 = sb("res1", [128, NT, 16], F32)
            posf = sb("posf", [128, NT, 16], F32)
            ps = [psb(f"ps{i}") for i in range(5)]
            psl = psb("psl")
            ptr = [psb(f"ptr{i}", (128, 1024), BF16) for i in range(2)]
            psi = [0]

            def nps():
                i = psi[0] % 5
                psi[0] += 1
                return ps[i], ("ps", i)

            def wload13(e_):
                b = e_ % 3
                for c in range(8):
                    P.ld("pool", w1[b][:, c, :], moe_w1[l, e_, c * 128:(c + 1) * 128, :], [], [("w1", b, c)])
                    P.ld("pool", w3[b][:, c, :], moe_w3[l, e_, c * 128:(c + 1) * 128, :], [], [("w3", b, c)])

            def wload2(e_):
                b = e_ % 2
                for c in range(8):
                    P.ld("pool", w2[b][:, c, :], moe_w2[l, e_, c * 128:(c + 1) * 128, :], [], [("w2", b, c)])

            wload13(0)
            wload2(0)
            P.op("pool", lambda e: e.iota(ioi, pattern=[[1, CAP]], base=0, channel_multiplier=0), [], ["ioi"])
            P.cp("dve", iof, ioi, ["ioi"], ["iof"])
            P.op("pool", lambda e: e.iota(tpi[:, :, 0:1], pattern=[[1, NT], [0, 1]], base=0, channel_multiplier=0), [], ["tpi"])
            P.op("pool", lambda e: e.iota(tpi[:, :, 1:2], pattern=[[0, NT], [0, 1]], base=0, channel_multiplier=1), [], ["tpi"])
            lo = sb("lo", [128, 16], F32)
            mid = sb("mid", [128, 16], F32)
            part = sb("part", [128, 16], F32)
            ge = sb("ge", [128, 16], F32)
            NF = NT * 16
            ysbf = ysb.rearrange("p g d -> p (g d)")
            cmpt = ysbf[:, 0:NF].rearrange("p (t e) -> p t e", e=16)
            cA = ysbf[:, NF:2 * NF].rearrange("p (t e) -> p t e", e=16)
            cB = ysbf[:, 2 * NF:3 * NF].rearrange("p (t e) -> p t e", e=16)
            p1 = posf
            P.op("dve", lambda e: e.memset(lo, 0.0), [], ["lo"])
            for k in range(NITER):
                ck = 2.0 ** -(k + 1)
                P.ts("dve", mid, lo, ck, None, ALU.add, None, ["lo"], ["thr"])
                P.tt("dve", cmpt, aff, mid.unsqueeze(1).broadcast_to([128, NT, 16]), ALU.is_ge, ["thr"], ["cmpt"])
                P.op("dve", lambda e: e.tensor_reduce(out=part, in_=cmpt.rearrange("p t e -> p e t"), axis=AX.X, op=ALU.add),
                     ["cmpt"], ["part"])
                pp, pk = nps()
                P.mm(pp[:, 0:16], onesf, part, True, True, ["part", "onesf"], [pk])
                P.ts("dve", ge, pp[:, 0:16], CAP - 0.5, None, ALU.is_ge, None, [pk], ["ge"])
                P.stt(lo, ge, ck, lo, ALU.mult, ALU.add, ["ge", "lo"], ["lo"])
            P.tt("dve", cmpt, aff, lo.unsqueeze(1).broadcast_to([128, NT, 16]), ALU.is_ge, ["lo"], ["cmpt"])
            cm2 = cmpt.rearrange("p t e -> p (t e)")
            pp1, pk1 = nps()
            P.mm(pp1[:, 0:NF], lstr, cm2, True, True, ["cmpt", "lstr"], [pk1])
            pp2, pk2 = nps()
            P.mm(pp2[:, 0:NF], onesf, cm2, True, True, ["cmpt", "onesf"], [pk2])
            P.cp("dve", p1.rearrange("p t e -> p (t e)"), pp1[:, 0:NF], [pk1], ["p1"])
            P.cp("dve", cA.rearrange("p t e -> p (t e)"), pp2[:, 0:NF], [pk2], ["cA"])
            cur, nxt, ck_, nk_ = cA, cB, "cA", "cB"
            s_ = 1
            while s_ < NT:
                P.tt("dve", nxt[:, s_:, :], cur[:, s_:, :], cur[:, 0:NT - s_, :], ALU.add, [ck_], [nk_])
                P.cp("dve", nxt[:, 0:s_, :], cur[:, 0:s_, :], [ck_], [nk_])
                cur, nxt, ck_, nk_ = nxt, cur, nk_, ck_
                s_ *= 2
            P.tt("dve", p1, p1, cur, ALU.add, ["p1", ck_], ["p1"])
            P.tt("dve", p1.rearrange("p t e -> p (t e)"), p1.rearrange("p t e -> p (t e)"), pp2[:, 0:NF], ALU.subtract,
                 ["p1", pk2], ["p1"])
            P.ts("dve", cmpt, cmpt, -1.0e6, 1.0e6, ALU.mult, ALU.add, ["cmpt"], ["cmpt"])
            P.tt("dve", p1, p1, cmpt, ALU.add, ["p1", "cmpt"], ["p1"])
            Rf = Rt.rearrange("p t e k -> p (t e) k")
            P.cp("dve", Rt[:, :, :, 0], tpi[:, :, 0:1].broadcast_to([128, NT, 16]), ["tpi"], ["Rt"])
            P.cp("dve", Rt[:, :, :, 1], tpi[:, :, 1:2].broadcast_to([128, NT, 16]), ["tpi"], ["Rt"])
            P.cp("dve", ahi, aff, [], ["ahi"])
            P.tt("dve", res1, aff, ahi, ALU.subtract, ["ahi"], ["res1"])
            P.cp("dve", amid, res1, ["res1"], ["amid"])
            P.tt("dve", res1, res1, amid, ALU.subtract, ["res1", "amid"], ["res1"])
            P.cp("dve", Rt[:, :, :, 2], ahi, ["ahi"], ["Rt"])
            P.cp("dve", Rt[:, :, :, 3], amid, ["amid"], ["Rt"])
            P.cp("dve", Rt[:, :, :, 4], res1, ["res1"], ["Rt"])

            def list_step(e_, t):
                o = oh[t % 2]
                P.ts("dve", o, iof, p1[:, t, e_:e_ + 1], None, ALU.is_equal, None, ["iof", "p1"], [("oh", t % 2)])
                for g in range(NG):
                    P.mm(psl[:, g * 8:g * 8 + 5], o[:, g * 128:(g + 1) * 128], Rt[:, t, e_, :], t == 0 and g == 0,
                         t == NT - 1 and g == NG - 1, [("oh", t % 2), "Rt"], ["psl"])

            def list_fin(e_):
                b = e_ % 2
                P.cp("dve", lst, psl[:, 0:NG * 8].rearrange("p (g k) -> p g k", k=8)[:, :, 0:5], ["psl"], ["lst"])
                P.stt(idf, lst[:, :, 0], 128.0, lst[:, :, 1], ALU.mult, ALU.add, ["lst"], ["idf"])
                P.cp("dve", ids2[b], idf, ["idf"], [("ids", b)])
                P.tt("dve", vals2[b], lst[:, :, 2], lst[:, :, 3], ALU.add, ["lst"], [("vals", b)])
                P.tt("dve", vals2[b], vals2[b], lst[:, :, 4], ALU.add, ["lst", ("vals", b)], [("vals", b)])
                for g in range(NG):
                    P.dma("pool", lambda e, g=g, b=b: e.indirect_dma_start(
                        out=xgs2[b][:, g, :], out_offset=None, in_=h2rows,
                        in_offset=bass.IndirectOffsetOnAxis(ap=ids2[b][:, g:g + 1], axis=0),
                        bounds_check=P.reg(e, S - 1), oob_is_err=False), [("ids", b)], [("xgs", b)])

            def build_lists(e_):
                for t in range(NT):
                    list_step(e_, t)
                list_fin(e_)

            def sadd(e_):
                ids = ids2[e_ % 2]
                for g in range(NG):
                    P.dma("pool", lambda e, g=g, ids=ids: e.indirect_dma_start(
                        out=xs, out_offset=bass.IndirectOffsetOnAxis(ap=ids[:, g:g + 1], axis=0),
                        in_=ysb[:, g, :], in_offset=None, bounds_check=P.reg(e, S - 1), oob_is_err=True, compute_op=ALU.add),
                        [("ysb", g), ("ids", e_ % 2)], ["xs"])

            build_lists(0)
            wload13(1)
            wload2(1)
            wload13(2)
            tri = 0
            for e_ in range(16):
                b = e_ % 2
                xgs, vals = xgs2[b], vals2[b]
                for g in range(NG):
                    pt = ptr[tri % 2]
                    for c in range(8):
                        P.tr(pt[:, c * 128:(c + 1) * 128], xgs[:, g, c * 128:(c + 1) * 128], identb, [("xgs", b), "identb"],
                             [("ptr", tri % 2)])
                    P.cp("act" if g % 2 else "dve", xgT[:, :, g * 128:(g + 1) * 128], pt.rearrange("p (c n) -> p c n", c=8),
                         [("ptr", tri % 2)], ["xgT"])
                    tri += 1
                if e_ > 0:
                    sadd(e_ - 1)
                for fc in range(8):
                    pa, pka = nps()
                    pb_, pkb = nps()
                    for c in range(8):
                        P.mm(pa[:, 0:CAP], w1[e_ % 3][:, c, fc * 128:(fc + 1) * 128], xgT[:, c, :], c == 0, c == 7,
                             ["xgT", ("w1", e_ % 3, c)], [pka])
                    for c in range(8):
                        P.mm(pb_[:, 0:CAP], w3[e_ % 3][:, c, fc * 128:(fc + 1) * 128], xgT[:, c, :], c == 0, c == 7,
                             ["xgT", ("w3", e_ % 3, c)], [pkb])
                    P.act(s1, pa[:, 0:CAP], AF.Silu, [pka], ["s1"])
                    P.tt("dve", aT[:, fc, :], s1, pb_[:, 0:CAP], ALU.mult, ["s1", pkb], ["aT"])
                    if e_ + 1 < 16:
                        for t in range(fc * NT // 8, (fc + 1) * NT // 8):
                            list_step(e_ + 1, t)
                if e_ + 1 < 16:
                    list_fin(e_ + 1)
                if e_ + 3 < 16:
                    wload13(e_ + 3)
                for g in range(NG):
                    for hf in range(2):
                        pp, pk = nps()
                        for c in range(8):
                            P.mm(pp, aT[:, c, g * 128:(g + 1) * 128], w2[b][:, c, hf * 512:(hf + 1) * 512], c == 0, c == 7,
                                 ["aT", ("w2", b, c)], [pk])
                        P.ts("dve", ysb[:, g, hf * 512:(hf + 1) * 512], pp, vals[:, g:g + 1], None, ALU.mult, None,
                             [pk, ("vals", b), "p1"], [("ysb", g)])
                if e_ + 2 < 16:
                    wload2(e_ + 2)
            sadd(15)
            P.flush()

        with ExitStack() as es:
            def sb(name, shape, dt):
                return es.enter_context(nc.sbuf_tensor(f"p6_{l}_{name}", list(shape), dt)).ap()

            def psb(name, shape=(128, 512), dt=F32):
                return es.enter_context(nc.psum_tensor(f"p6_{l}_{name}", list(shape), dt)).ap()

            wg = sb("wg", [128, 8, D], BF16)
            wp = sb("wp", [128, 2, D], BF16)
            gple = sb("gple", [128, D], F32)
            gfin = sb("gfin", [128, D], F32)
            xb2 = [sb(f"xblk{i}", [128, 4, D], F32) for i in range(2)]
            pb2 = [sb(f"pblk{i}", [128, 4, 256], F32) for i in range(2)]
            pbf2 = [sb(f"pbf{i}", [128, 4, 256], BF16) for i in range(2)]
            junk = sb("junk", [128, D], BF16)
            ss2 = [sb(f"ss{i}", [128, 4], F32) for i in range(2)]
            rstd2 = [sb(f"rstd{i}", [128, 4], F32) for i in range(2)]
            ssf = sb("ssf", [128, 4], F32)
            rstdf = sb("rstdf", [128, 4], F32)
            h2b = [sb(f"h{i}", [128, 4, D], BF16) for i in range(2)]
            hT = sb("hT", [128, 8, 512], BF16)
            pT = sb("pT", [128, 2, 512], BF16)
            sg = sb("sg", [128, 512], F32)
            sg1 = sb("sg1", [128, 512], F32)
            yb = sb("yb", [128, 4, D], F32)
            ps = [psb(f"ps{i}") for i in range(6)]
            pst = [psb(f"pst{i}", (128, 1024), BF16) for i in range(2)]
            psi = [0]

            def nps():
                i = psi[0] % 6
                psi[0] += 1
                return ps[i], ("ps", i)

            for c in range(8):
                P.ld("pool", wg[:, c, :], ple_gate_w[l, c * 128:(c + 1) * 128, :], [], [("wg", c)])
            for c in range(2):
                P.ld("pool", wp[:, c, :], ple_w[l, c * 128:(c + 1) * 128, :], [], [("wp", c)])
            P.ld("sp", gple, norm_ple[l].partition_broadcast(128), [], ["gple"])
            P.ld("sp", gfin, norm_final[0].partition_broadcast(128), [], ["gfin"])
            last = (l == L - 1)

            def pro_a(blk):
                i = blk % 2
                tok0 = blk * 512
                xblk, pblk, pbf, ss, rstd, h = xb2[i], pb2[i], pbf2[i], ss2[i], rstd2[i], h2b[i]
                P.ld("sp", xblk, rows_view(xs, tok0, 4), [], [("xblk", i)])
                P.ld("sp", pblk, rows_view(p_in[l], tok0, 4), [], [("pblk", i)])
                for t in range(4):
                    P.act(junk, xblk[:, t, :], AF.Square, [("xblk", i)], ["junk", ("ss", i)], accum_out=ss[:, t:t + 1])
                P.ts("dve", rstd, ss, 1.0 / D, EPS, ALU.mult, ALU.add, [("ss", i)], [("rstd", i)])
                P.op("act", lambda e: e.sqrt(out=rstd, in_=rstd), [("rstd", i)], [("rstd", i)])
                P.op("dve", lambda e: e.reciprocal(out=rstd, in_=rstd), [("rstd", i)], [("rstd", i)])
                P.cp("pool", pbf, pblk, [("pblk", i)], [("pbf", i)])
                for t in range(4):
                    P.stt(h[:, t, :], xblk[:, t, :], rstd[:, t:t + 1], gple, ALU.mult, ALU.mult,
                          [("xblk", i), ("rstd", i), "gple"], [("h", i, t)])

            def pro_b(blk):
                i = blk % 2
                h, pbf = h2b[i], pbf2[i]
                for t in range(4):
                    pt = pst[t % 2]
                    for c in range(8):
                        P.tr(pt[:, c * 128:(c + 1) * 128], h[:, t, c * 128:(c + 1) * 128], identb,
                             [("h", i, t), "identb"], [("pst", t % 2)])
                    P.cp("act" if t % 2 else "dve", hT[:, :, t * 128:(t + 1) * 128],
                         pt.rearrange("p (c n) -> p c n", c=8), [("pst", t % 2)], ["hT"])
                for t in range(4):
                    pt = pst[t % 2]
                    for c in range(2):
                        P.tr(pt[:, c * 128:(c + 1) * 128], pbf[:, t, c * 128:(c + 1) * 128], identb,
                             [("pbf", i), "identb"], [("pst", t % 2)])
                    P.cp("act" if t % 2 else "dve", pT[:, :, t * 128:(t + 1) * 128],
                         pt[:, 0:256].rearrange("p (c n) -> p c n", c=2), [("pst", t % 2)], ["pT"])

            pro_a(0)
            pro_b(0)
            for blk in range(NB):
                tok0 = blk * 512
                bi = blk % 2
                xblk = xb2[bi]
                xk = ("xblk", bi)
                if blk + 1 < NB:
                    pro_a(blk + 1)
                for t in range(4):
                    for hf in range(2):
                        pg, pkg = nps()
                        pl, pkl = nps()
                        for c in range(8):
                            P.mm(pg, hT[:, c, t * 128:(t + 1) * 128], wg[:, c, hf * 512:(hf + 1) * 512], c == 0, c == 7,
                                 ["hT", ("wg", c)], [pkg])
                        for c in range(2):
                            P.mm(pl, pT[:, c, t * 128:(t + 1) * 128], wp[:, c, hf * 512:(hf + 1) * 512], c == 0, c == 1,
                                 ["pT", ("wp", c)], [pkl])
                        sg_ = sg if hf == 0 else sg1
                        sk = ("sg", hf)
                        P.act(sg_, pg, AF.Sigmoid, [pkg], [sk])
                        P.tt("dve", sg_, sg_, pl, ALU.mult, [sk, pkl], [sk])
                        P.tt("pool", xblk[:, t, hf * 512:(hf + 1) * 512], xblk[:, t, hf * 512:(hf + 1) * 512], sg_, ALU.add,
                             [sk, xk], [xk])
                if blk + 1 < NB:
                    pro_b(blk + 1)
                if not last:
                    P.ld("sp", rows_view(xs, tok0, 4), xblk, [xk], ["xs"])
                else:
                    for t in range(4):
                        P.act(junk, xblk[:, t, :], AF.Square, [xk], ["junk", "ssf"], accum_out=ssf[:, t:t + 1])
                    P.ts("dve", rstdf, ssf, 1.0 / D, EPS, ALU.mult, ALU.add, ["ssf"], ["rstdf"])
                    P.op("act", lambda e: e.sqrt(out=rstdf, in_=rstdf), ["rstdf"], ["rstdf"])
                    P.op("dve", lambda e: e.reciprocal(out=rstdf, in_=rstdf), ["rstdf"], ["rstdf"])
                    for t in range(4):
                        P.stt(yb[:, t, :], xblk[:, t, :], rstdf[:, t:t + 1], gfin, ALU.mult, ALU.mult,
                              [xk, "rstdf", "gfin"], ["yb"])
                    P.ld("sp", rows_view(y_out, tok0, 4), yb, ["yb"], ["y"])
            P.flush()
    return nc


def _host_layout(inputs, S, L):
    f = np.float32
    w_in = np.asarray(inputs["w_in"], f)[:L]
    kr = 2176
    swap = np.concatenate([np.arange(8, 16), np.arange(0, 8), np.arange(24, 32), np.arange(16, 24)])
    w_in_ext = np.concatenate([w_in, w_in[:, :, 2112:2176], w_in[:, :, kr + swap]], axis=2)
    wq = np.asarray(inputs["mla_wq_up"], f)[:L].reshape(L, 384, 8, 96)
    wq_ext = np.concatenate([wq, wq[..., 0:64], wq[..., 64 + swap]], axis=3).reshape(L, 384, 8 * 192)
    wkv = np.asarray(inputs["mla_wkv_up"], f)[:L].reshape(L, 256, 8, 128)
    wkv_re = np.concatenate([wkv[..., 0:64].reshape(L, 256, 512), wkv[..., 64:128].reshape(L, 256, 512)], axis=2)
    rpb = np.asarray(inputs["na_rpb"], f)[:L]
    kc = np.arange(64)[:, None]
    qc = np.arange(64)[None, :]
    dc = np.clip(kc - qc + 15, 0, 30)
    tz = np.empty((L, 128, 8, 14, 64), f)
    for krel in range(2):
        for d in range(14):
            tz[:, krel * 64:(krel + 1) * 64, :, d, :] = np.transpose(rpb[:, :, d + krel, :][:, :, dc], (0, 2, 1, 3))
    col_start = np.clip(np.arange(64) - 8, 0, 48)[None, :]
    valid = (kc >= col_start) & (kc < col_start + 16)
    colmask = np.where(valid, 0.0, MASKV).astype(f)
    colmask = np.concatenate([colmask, colmask], axis=0)
    t = np.arange(S)
    freqs = (1.0 / (10000.0 ** (np.arange(0, 16, 2, dtype=f) / f(16)))).astype(f)
    cos = np.empty((32, S), f)
    sin = np.empty((32, S), f)
    for j in range(32):
        pos = (t // 64) if j < 16 else (t % 64)
        jj = j % 16
        ang = pos.astype(f) * freqs[jj % 8]
        cos[j] = np.cos(ang)
        sin[j] = np.sin(ang) * (-1.0 if jj < 8 else 1.0)
    rope = np.stack([cos, sin]).astype(f)
    shared = {
        "norm_mix": inputs["norm_mix"][:L], "w_in_ext": w_in_ext, "tz": tz.reshape(L, 128, -1), "colmask": colmask,
        "rope": rope, "mla_q_norm": inputs["mla_q_norm"][:L], "wq_ext": wq_ext, "mla_kv_norm": inputs["mla_kv_norm"][:L],
        "wkv_re": wkv_re, "w_na_o": inputs["w_na_o"][:L], "w_mla_o": inputs["w_mla_o"][:L], "w_out": inputs["w_out"][:L],
        "norm_moe": inputs["norm_moe"][:L], "w_router": inputs["w_router"][:L], "moe_w1": inputs["moe_w1"][:L],
        "moe_w3": inputs["moe_w3"][:L], "moe_w2": inputs["moe_w2"][:L], "norm_ple": inputs["norm_ple"][:L],
        "ple_gate_w": inputs["ple_gate_w"][:L], "ple_w": inputs["ple_w"][:L],
        "norm_final": np.asarray(inputs["norm_final"], f).reshape(1, D),
    }
    return {k: np.ascontiguousarray(np.asarray(v, f)) for k, v in shared.items()}


def kernel(**inputs):
    x = np.asarray(inputs["x"], np.float32)
    p = np.asarray(inputs["p"], np.float32)
    B, S, _ = x.shape
    L = p.shape[0]
    nc = build(S, L)
    shared = _host_layout(inputs, S, L)
    in_maps = []
    for b in range(B):
        m = dict(shared)
        m["x"] = np.ascontiguousarray(x[b])
        m["p"] = np.ascontiguousarray(p[:, b])
        in_maps.append(m)
    res = run_bass_kernel_spmd(nc, in_maps, core_ids=list(range(B)))
    return np.stack([np.asarray(r["y"], np.float32) for r in res.results], axis=0)
```

```python
from contextlib import ExitStack
import numpy as np
import concourse.bass as bass
import concourse.mybir as mybir
from concourse.bass_utils import run_bass_kernel_spmd

F32 = mybir.dt.float32
BF16 = mybir.dt.bfloat16
I32 = mybir.dt.int32
AF = mybir.ActivationFunctionType
ALU = mybir.AluOpType
AX = mybir.AxisListType

D = 1024
NDMA_SEM = 24
EPS = 1e-6
MASKV = -240000.0


class Prog:
    ENGS = ("pe", "dve", "act", "pool", "sp")

    def __init__(self, nc):
        self.nc = nc
        self.stream = {e: [] for e in self.ENGS}
        self.cnt = {e: 0 for e in self.ENGS}
        self.sem = {}
        self.known = {e: {} for e in self.ENGS}
        self.last_w = {}
        self.readers = {}
        self.dq = {}
        self._ctx = []
        for e in ("pe", "dve", "act", "pool"):
            self.sem[e] = self._mksem("c_" + e)
        for q in ("sp", "pool", "act"):
            self.dq[q] = {"sems": [self._mksem(f"d_{q}{i}") for i in range(NDMA_SEM)], "n": 0}
        self.nops = 0

    def _mksem(self, name):
        cm = self.nc.semaphore(name)
        s = cm.__enter__()
        self._ctx.append(cm)
        return s

    def _need(self, eng, ev):
        sem, val, src, is_dma = ev
        if (not is_dma) and src == eng and eng == "pe":
            return
        k = self.known[eng]
        if k.get(id(sem), 0) >= val:
            return
        k[id(sem)] = val
        self.stream[eng].append(("wait", sem, val))

    def _deps(self, eng, reads, writes):
        for key in reads:
            ev = self.last_w.get(key)
            if ev is not None:
                self._need(eng, ev)
        for key in writes:
            ev = self.last_w.get(key)
            if ev is not None:
                self._need(eng, ev)
            for ev in self.readers.get(key, ()):
                self._need(eng, ev)

    def _commit(self, ev, reads, writes):
        for key in reads:
            self.readers.setdefault(key, []).append(ev)
        for key in writes:
            self.last_w[key] = ev
            self.readers[key] = []

    def op(self, eng, fn, reads=(), writes=()):
        self._deps(eng, reads, writes)
        self.cnt[eng] += 1
        ev = (self.sem[eng], self.cnt[eng], eng, False)
        self.stream[eng].append(("op", fn, self.sem[eng], 1))
        self._commit(ev, reads, writes)
        self.nops += 1
        return ev

    def dma(self, q, fn, reads=(), writes=()):
        d = self.dq[q]
        i = d["n"]
        d["n"] += 1
        sem = d["sems"][i % NDMA_SEM]
        rnd = i // NDMA_SEM
        if rnd > 0:
            self._need(q, (sem, 16 * rnd, q, True))
        self._deps(q, reads, writes)
        ev = (sem, 16 * (rnd + 1), q, True)
        self.stream[q].append(("op", fn, sem, 16))
        self._commit(ev, reads, writes)
        self.nops += 1
        return ev

    def barrier(self):
        for e in self.ENGS:
            for e2 in ("pe", "dve", "act", "pool"):
                if self.cnt[e2] > 0:
                    self._need(e, (self.sem[e2], self.cnt[e2], e2, False))
            for q, d in self.dq.items():
                for j, sem in enumerate(d["sems"]):
                    n = (d["n"] - j + NDMA_SEM - 1) // NDMA_SEM if d["n"] > j else 0
                    if n > 0:
                        self._need(e, (sem, 16 * n, q, True))
        self.last_w = {}
        self.readers = {}

    def flush(self):
        self.barrier()
        nc = self.nc
        emap = {"pe": "tensor", "dve": "vector", "act": "scalar", "pool": "gpsimd", "sp": "sync"}
        with nc.Block() as block:
            for e in self.ENGS:
                items = self.stream[e]

                def body(eng, items=items):
                    self.regcache = {}
                    for it in items:
                        if it[0] == "wait":
                            eng.wait_ge(it[1], it[2])
                        else:
                            it[1](eng).then_inc(it[2], it[3])

                getattr(block, emap[e])(body)
        self.stream = {e: [] for e in self.ENGS}

    def reg(self, eng, val):
        if val not in self.regcache:
            self.regcache[val] = eng.to_reg(val)
        return self.regcache[val]

    def mm(self, out, lhsT, rhs, start, stop, reads, writes):
        return self.op("pe", lambda e: e.matmul(out, lhsT=lhsT, rhs=rhs, start=start, stop=stop), reads, writes)

    def tr(self, out, in_, ident, reads, writes):
        return self.op("pe", lambda e: e.transpose(out=out, in_=in_, identity=ident), reads, writes)

    def act(self, out, in_, func, reads, writes, scale=1.0, accum_out=None):
        if accum_out is None:
            return self.op("act", lambda e: e.activation(out=out, in_=in_, func=func, scale=scale), reads, writes)
        return self.op("act", lambda e: e.activation(out=out, in_=in_, func=func, scale=scale, accum_out=accum_out),
                       reads, writes)

    def tt(self, eng, out, in0, in1, op, reads, writes):
        return self.op(eng, lambda e: e.tensor_tensor(out=out, in0=in0, in1=in1, op=op), reads, writes)

    def ts(self, eng, out, in0, s1, s2, op0, op1, reads, writes):
        if op1 is None:
            return self.op(eng, lambda e: e.tensor_scalar(out=out, in0=in0, scalar1=s1, scalar2=None, op0=op0),
                           reads, writes)
        return self.op(eng, lambda e: e.tensor_scalar(out=out, in0=in0, scalar1=s1, scalar2=s2, op0=op0, op1=op1),
                       reads, writes)

    def stt(self, out, in0, scalar, in1, op0, op1, reads, writes):
        return self.op("dve", lambda e: e.scalar_tensor_tensor(out=out, in0=in0, scalar=scalar, in1=in1, op0=op0, op1=op1),
                       reads, writes)

    def cp(self, eng, out, in_, reads, writes):
        if eng == "act":
            return self.op("act", lambda e: e.copy(out=out, in_=in_), reads, writes)
        return self.op(eng, lambda e: e.tensor_copy(out=out, in_=in_), reads, writes)

    def ld(self, q, out, in_, reads, writes):
        return self.dma(q, lambda e: e.dma_start(out=out, in_=in_), reads, writes)


def build(S=4096, L=2, dbg=()):
    NT = S // 128
    NB = S // 512
    ROWS = S // 64
    CAP = 2 * S // 16
    NG = CAP // 128
    NITER = 30
    nc = bass.Bass("TRN2", target_bir_lowering=False)
    P = Prog(nc)

    def din(name, shape, dt=F32):
        return nc.dram_tensor(name, list(shape), dt, kind="ExternalInput").ap()

    def dscr(name, shape, dt):
        return nc.dram_tensor(name, list(shape), dt).ap()

    x_in = din("x", [S, D])
    p_in = din("p", [L, S, 256])
    norm_mix = din("norm_mix", [L, D])
    w_in = din("w_in_ext", [L, D, 4352])
    tz_in = din("tz", [L, 128, 8 * 14 * 64])
    colmask_in = din("colmask", [128, 64])
    rope_in = din("rope", [2, 32, S])
    q_norm = din("mla_q_norm", [L, 384])
    wq_in = din("wq_ext", [L, 384, 8 * 192])
    kv_norm = din("mla_kv_norm", [L, 256])
    wkv_in = din("wkv_re", [L, 256, 1024])
    w_na_o = din("w_na_o", [L, 512, D])
    w_mla_o = din("w_mla_o", [L, 512, D])
    w_out = din("w_out", [L, D, D])
    norm_moe = din("norm_moe", [L, D])
    w_router = din("w_router", [L, D, 16])
    moe_w1 = din("moe_w1", [L, 16, D, D])
    moe_w3 = din("moe_w3", [L, 16, D, D])
    moe_w2 = din("moe_w2", [L, 16, D, D])
    norm_ple = din("norm_ple", [L, D])
    ple_gate_w = din("ple_gate_w", [L, D, D])
    ple_w = din("ple_w", [L, 256, D])
    norm_final = din("norm_final", [1, D])
    y_out = nc.dram_tensor("y", [S, D], F32, kind="ExternalOutput").ap()

    xs = dscr("xs", [S, D], F32)
    qnT = dscr("qnT", [512, S], BF16)
    knT = dscr("knT", [512, S], BF16)
    vn_aug = dscr("vn_aug", [S, 520], BF16)
    qmT = dscr("qmT", [8, 96, S], BF16)
    kmT = dscr("kmT", [8, 64, S], BF16)
    kpeT = dscr("kpeT", [32, S], BF16)
    vm_aug = dscr("vm_aug", [S, 584], BF16)
    gaT = dscr("gaT", [D, S], BF16)
    gbT = dscr("gbT", [D, S], BF16)
    naT = dscr("naT", [512, S], BF16)
    mlaT = dscr("mlaT", [512, S], BF16)
    RW = 1152
    h2rows = dscr("h2rows", [S, RW], BF16)
    xg = [dscr(f"xg{e}", [CAP, RW], BF16) for e in range(16)]
    dbg_t = {}
    for name, shape, dt in dbg:
        dbg_t[name] = nc.dram_tensor("dbg_" + name, list(shape), dt, kind="ExternalOutput").ap()

    def palloc(name, shape, dt):
        return nc.alloc_sbuf_tensor(name, list(shape), dt).ap()

    identf = palloc("identf", [128, 128], F32)
    identb = palloc("identb", [128, 128], BF16)
    onesf = palloc("onesf", [128, 128], F32)
    lstr = palloc("lstr", [128, 128], F32)
    posi = palloc("posi", [128, NT, 16], I32)
    aff = palloc("aff", [128, NT, 16], F32)
    P.op("pool", lambda e: e.memset(identf, 0.0), writes=["identf"])
    P.op("pool", lambda e: e.affine_select(out=identf, in_=identf, pattern=[[-1, 128]], compare_op=ALU.not_equal,
                                           fill=1.0, base=0, channel_multiplier=1), ["identf"], ["identf"])
    P.cp("dve", identb, identf, ["identf"], ["identb"])
    P.op("pool", lambda e: e.memset(onesf, 1.0), writes=["onesf"])
    P.op("pool", lambda e: e.memset(lstr, 1.0), writes=["lstr"])
    P.op("pool", lambda e: e.affine_select(out=lstr, in_=lstr, pattern=[[1, 128]], compare_op=ALU.is_gt,
                                           fill=0.0, base=0, channel_multiplier=-1), ["lstr"], ["lstr"])
    P.flush()

    def blkview(ap2d, blk):
        return ap2d.rearrange("(c p) s -> p c s", p=128)[:, :, blk * 512:(blk + 1) * 512]

    def rows_view(ap2d, r0, nt):
        return ap2d[r0:r0 + 128 * nt, :].rearrange("(t p) f -> p t f", p=128)

    hT_d = dscr("hT_d", [D, S], BF16)

    for l in range(L):
        xsrc = x_in if l == 0 else xs

        with ExitStack() as es:
            def sb(name, shape, dt):
                return es.enter_context(nc.sbuf_tensor(f"p1_{l}_{name}", list(shape), dt)).ap()

            def psb(name, shape=(128, 512), dt=F32):
                return es.enter_context(nc.psum_tensor(f"p1_{l}_{name}", list(shape), dt)).ap()

            win = sb("win", [128, 8, 2304], BF16)
            wq = sb("wq", [128, 3, 1536], BF16)
            wkv = sb("wkv", [128, 2, 1024], BF16)
            gmix = sb("gmix", [128, D], F32)
            gq = sb("gq", [128, 3], F32)
            gkv = sb("gkv", [128, 2], F32)
            xb2 = [sb(f"xblk{i}", [128, 4, D], F32) for i in range(2)]
            junk = sb("junk", [128, D], BF16)
            ss2 = [sb(f"ss{i}", [128, 4], F32) for i in range(2)]
            rstd2 = [sb(f"rstd{i}", [128, 4], F32) for i in range(2)]
            h2b = [sb(f"h{i}", [128, 4, D], BF16) for i in range(2)]
            hT = sb("hT", [128, 8, 512], BF16)
            stq = sb("stq", [128, 4, 512], BF16)
            stk = sb("stk", [128, 4, 512], BF16)
            stv = sb("stv", [128, 4, 520], BF16)
            stv2 = sb("stv2", [128, 4, 584], BF16)
            qlat = sb("qlat", [128, 3, 512], F32)
            kvlat = sb("kvlat", [128, 2, 512], F32)
            sq = sb("sq", [128, 3, 512], F32)
            rt = sb("rt", [128, 512], F32)
            qn_ = sb("qn_", [128, 3, 512], BF16)
            kvn_ = sb("kvn_", [128, 2, 512], BF16)
            cs2 = [sb(f"cs{i}", [128, 2, 512], F32) for i in range(2)]
            t1 = sb("t1", [128, 512], F32)
            t2 = sb("t2", [128, 512], F32)
            kpest = sb("kpest", [128, 512], BF16)
            qst = sb("qst", [128, 8, 512], BF16)
            kst = sb("kst", [128, 8, 512], BF16)
            ps = [psb(f"ps{i}") for i in range(6)]
            pst = [psb(f"pst{i}", (128, 1024), BF16) for i in range(2)]
            psi = [0]

            def nps():
                i = psi[0] % 6
                psi[0] += 1
                return ps[i], ("ps", i)

            for c in range(8):
                rs = slice(c * 128, (c + 1) * 128)
                P.ld("pool", win[:, c, 0:1024], w_in[l, rs, 0:1024], [], [("win", c, 0)])
                P.ld("pool", win[:, c, 1024:2048], w_in[l, rs, 1024:2048], [], [("win", c, 1)])
                P.ld("pool", win[:, c, 2048:2208], w_in[l, rs, 2048:2208], [], [("win", c, 2)])
                P.ld("pool", win[:, c, 2208:2304], w_in[l, rs, 4256:4352], [], [("win", c, 3)])
            for c in range(3):
                for j in range(2):
                    P.ld("pool", wq[:, c, j * 768:(j + 1) * 768], wq_in[l, c * 128:(c + 1) * 128, j * 768:(j + 1) * 768],
                         [], [("wq", c)] if j == 1 else [("wq0", c)])
            for c in range(2):
                P.ld("pool", wkv[:, c, :], wkv_in[l, c * 128:(c + 1) * 128, :], [], [("wkv", c)])
            P.ld("sp", gmix, norm_mix[l].partition_broadcast(128), [], ["gmix"])
            for c in range(3):
                P.ld("sp", gq[:, c:c + 1], q_norm[l, c * 128:(c + 1) * 128].rearrange("(p o) -> p o", o=1), [], ["gq"])
            for c in range(2):
                P.ld("sp", gkv[:, c:c + 1], kv_norm[l, c * 128:(c + 1) * 128].rearrange("(p o) -> p o", o=1), [], ["gkv"])
            P.op("pool", lambda e: e.memset(stv, 1.0), writes=["stv"])
            P.op("pool", lambda e: e.memset(stv2, 1.0), writes=["stv2"])

            def wkeys(c, col0, m):
                f = lambda col: 3 if col >= 2208 else col // 1024
                return list({("win", c, f(col0)), ("win", c, f(col0 + m - 1))})

            def pro_a(blk):
                i = blk % 2
                tok0 = blk * 512
                xblk, cs, ss, rstd, h = xb2[i], cs2[i], ss2[i], rstd2[i], h2b[i]
                P.ld("sp", xblk, rows_view(xsrc, tok0, 4), [], [("xblk", i)])
                P.ld("sp", cs[64:96, 0, :], rope_in[0, :, tok0:tok0 + 512], [], [("cs", i)])
                P.ld("sp", cs[64:96, 1, :], rope_in[1, :, tok0:tok0 + 512], [], [("cs", i)])
                for t in range(4):
                    P.act(junk, xblk[:, t, :], AF.Square, [("xblk", i)], ["junk", ("ss", i)], accum_out=ss[:, t:t + 1])
                P.ts("dve", rstd, ss, 1.0 / D, EPS, ALU.mult, ALU.add, [("ss", i)], [("rstd", i)])
                P.op("act", lambda e: e.sqrt(out=rstd, in_=rstd), [("rstd", i)], [("rstd", i)])
                P.op("dve", lambda e: e.reciprocal(out=rstd, in_=rstd), [("rstd", i)], [("rstd", i)])
                for t in range(4):
                    P.stt(h[:, t, :], xblk[:, t, :], rstd[:, t:t + 1], gmix, ALU.mult, ALU.mult,
                          [("xblk", i), ("rstd", i), "gmix"], [("h", i, t)])

            def pro_b(blk):
                i = blk % 2
                h = h2b[i]
                for t in range(4):
                    pt = pst[t % 2]
                    for c in range(8):
                        P.tr(pt[:, c * 128:(c + 1) * 128], h[:, t, c * 128:(c + 1) * 128], identb,
                             [("h", i, t), "identb"], [("pst", t % 2)])
                    P.cp("act" if t % 2 else "dve", hT[:, :, t * 128:(t + 1) * 128],
                         pt.rearrange("p (c n) -> p c n", c=8), [("pst", t % 2)], ["hT"])
                P.ld("sp", blkview(hT_d, blk), hT, ["hT"], ["hT_d"])

            pro_a(0)
            pro_b(0)
            for blk in range(NB):
                tok0 = blk * 512
                cs = cs2[blk % 2]
                csk = ("cs", blk % 2)
                if blk + 1 < NB:
                    pro_a(blk + 1)

                def fm_group(col0, m):
                    pp, pk = nps()
                    for c in range(8):
                        P.mm(pp[0:m, :], win[:, c, col0:col0 + m], hT[:, c, :], c == 0, c == 7,
                             ["hT"] + wkeys(c, col0, m), [pk])
                    return pp, pk

                def lat_sq(src, nch, tag):
                    for c in range(nch):
                        P.act(sq[:, c, :], src[:, c, :], AF.Square, [tag], ["sq"])

                def lat_fin(src, nch, dim, gvec, gkey, dst, tag):
                    pp, pk = nps()
                    for c in range(nch):
                        P.mm(pp, onesf, sq[:, c, :], c == 0, c == nch - 1, ["sq", "onesf"], [pk])
                    P.ts("dve", rt, pp, 1.0 / dim, EPS, ALU.mult, ALU.add, [pk], ["rt"])
                    P.op("act", lambda e: e.sqrt(out=rt, in_=rt), ["rt"], ["rt"])
                    P.op("dve", lambda e: e.reciprocal(out=rt, in_=rt), ["rt"], ["rt"])
                    for c in range(nch):
                        P.stt(dst[:, c, :], src[:, c, :], gvec[:, c:c + 1], rt, ALU.mult, ALU.mult,
                              [tag, "rt", gkey], [tag + "n"])

                for j in range(3):
                    pp, pk = fm_group(1536 + j * 128, 128)
                    P.cp("act", qlat[:, j, :], pp, [pk], ["qlat"])
                for j in range(2):
                    pp, pk = fm_group(1920 + j * 128, 128)
                    P.cp("dve", kvlat[:, j, :], pp, [pk], ["kvlat"])
                lat_sq(qlat, 3, "qlat")
                for j in range(4):
                    pp, pk = fm_group(j * 128, 128)
                    P.cp("act", stq[:, j, :], pp, [pk], ["stq"])
                P.ld("sp", blkview(qnT, blk), stq, ["stq"], ["qnT"])
                lat_fin(qlat, 3, 384, gq, "gq", qn_, "qlat")
                lat_sq(kvlat, 2, "kvlat")
                for j in range(4):
                    pp, pk = fm_group(512 + j * 128, 128)
                    P.cp("dve", stk[:, j, :], pp, [pk], ["stk"])
                P.ld("sp", blkview(knT, blk), stk, ["stk"], ["knT"])
                lat_fin(kvlat, 2, 256, gkv, "gkv", kvn_, "kvlat")
                for t in range(4):
                    pp, pk = nps()
                    for c in range(8):
                        P.mm(pp, hT[:, c, t * 128:(t + 1) * 128], win[:, c, 1024:1536], c == 0, c == 7,
                             ["hT", ("win", c, 1)], [pk])
                    P.cp("act", stv[:, t, :].rearrange("p (h d) -> p h d", d=65)[:, :, 0:64],
                         pp.rearrange("p (h d) -> p h d", d=64), [pk], ["stv"])
                P.ld("sp", rows_view(vn_aug, tok0, 4), stv, ["stv"], ["vn_aug"])
                ppa, pka = fm_group(2112, 96)
                ppb, pkb = fm_group(2208, 96)
                P.tt("dve", t1[64:96, :], ppa[64:96, :], cs[64:96, 0, :], ALU.mult, [pka, csk], ["t1"])
                P.tt("dve", t2[64:96, :], ppb[64:96, :], cs[64:96, 1, :], ALU.mult, [pkb, csk], ["t2"])
                P.tt("pool", kpest[64:96, :], t1[64:96, :], t2[64:96, :], ALU.add, ["t1", "t2"], ["kpest"])
                P.ld("sp", kpeT[:, tok0:tok0 + 512], kpest[64:96, :], ["kpest"], ["kpeT"])
                if blk + 1 < NB:
                    pro_b(blk + 1)

                for hh in range(8):
                    ppa, pka = nps()
                    ppb, pkb = nps()
                    for c in range(3):
                        P.mm(ppa[0:96, :], wq[:, c, hh * 192:hh * 192 + 96], qn_[:, c, :], c == 0, c == 2,
                             ["qlatn", ("wq", c), ("wq0", c)], [pka])
                    for c in range(3):
                        P.mm(ppb[0:96, :], wq[:, c, hh * 192 + 96:hh * 192 + 192], qn_[:, c, :], c == 0, c == 2,
                             ["qlatn", ("wq", c), ("wq0", c)], [pkb])
                    P.cp("act", qst[0:64, hh, :], ppa[0:64, :], [pka], ["qst"])
                    P.tt("dve", t1[64:96, :], ppa[64:96, :], cs[64:96, 0, :], ALU.mult, [pka, csk], ["t1"])
                    P.tt("dve", t2[64:96, :], ppb[64:96, :], cs[64:96, 1, :], ALU.mult, [pkb, csk], ["t2"])
                    P.tt("pool", qst[64:96, hh, :], t1[64:96, :], t2[64:96, :], ALU.add, ["t1", "t2"], ["qst"])
                P.ld("sp", qmT.rearrange("h r s -> r h s")[:, :, tok0:tok0 + 512], qst[0:96], ["qst"], ["qmT"])
                for hh in range(8):
                    pp, pk = nps()
                    for c in range(2):
                        P.mm(pp[0:64, :], wkv[:, c, hh * 64:(hh + 1) * 64], kvn_[:, c, :], c == 0, c == 1,
                             ["kvlatn", ("wkv", c)], [pk])
                    P.cp("act" if hh % 2 else "dve", kst[0:64, hh, :], pp[0:64, :], [pk], ["kst"])
                P.ld("sp", kmT.rearrange("h r s -> r h s")[:, :, tok0:tok0 + 512], kst[0:64], ["kst"], ["kmT"])
                for t in range(4):
                    pp, pk = nps()
                    for c in range(2):
                        P.mm(pp, kvn_[:, c, t * 128:(t + 1) * 128], wkv[:, c, 512:1024], c == 0, c == 1,
                             ["kvlatn", ("wkv", c)], [pk])
                    P.cp("act", stv2[:, t, 0:520].rearrange("p (h d) -> p h d", d=65)[:, :, 0:64],
                         pp.rearrange("p (h d) -> p h d", d=64), [pk], ["stv2"])
                P.ld("sp", rows_view(vm_aug, tok0, 4), stv2, ["stv2"], ["vm_aug"])
            P.flush()

        with ExitStack() as es:
            def sb(name, shape, dt):
                return es.enter_context(nc.sbuf_tensor(f"p1b_{l}_{name}", list(shape), dt)).ap()

            def psb(name, shape=(128, 512), dt=F32):
                return es.enter_context(nc.psum_tensor(f"p1b_{l}_{name}", list(shape), dt)).ap()

            wing = sb("wing", [128, 8, 2048], BF16)
            hTb = [sb(f"hTb{i}", [128, 8, 512], BF16) for i in range(2)]
            stg = [sb(f"stg{i}", [128, 8, 512], BF16) for i in range(2)]
            ps = [psb(f"ps{i}") for i in range(8)]
            for c in range(8):
                for j in range(2):
                    P.ld("pool", wing[:, c, j * 1024:(j + 1) * 1024],
                         w_in[l, c * 128:(c + 1) * 128, 2208 + j * 1024:2208 + (j + 1) * 1024], [], [("wing", c, j)])
            gi_ = 0
            P.ld("sp", hTb[0], blkview(hT_d, 0), [], [("hTb", 0)])
            for blk in range(NB):
                hb = hTb[blk % 2]
                if blk + 1 < NB:
                    P.ld("sp", hTb[(blk + 1) % 2], blkview(hT_d, blk + 1), [], [("hTb", (blk + 1) % 2)])
                for gi, gdst in enumerate((gaT, gbT)):
                    sg_ = stg[gi]
                    for j in range(8):
                        pp, pk = ps[gi_ % 8], ("ps", gi_ % 8)
                        gi_ += 1
                        col0 = gi * 1024 + j * 128
                        for c in range(8):
                            P.mm(pp, wing[:, c, col0:col0 + 128], hb[:, c, :], c == 0, c == 7,
                                 [("hTb", blk % 2), ("wing", c, gi)], [pk])
                        P.act(sg_[:, j, :], pp, AF.Sigmoid, [pk], [("stg", gi)])
                    P.ld("sp", blkview(gdst, blk), sg_, [("stg", gi)], [("gT", gi)])
            P.flush()

        with ExitStack() as es:
            def sb(name, shape, dt):
                return es.enter_context(nc.sbuf_tensor(f"p2_{l}_{name}", list(shape), dt)).ap()

            def psb(name, shape=(128, 512), dt=F32):
                return es.enter_context(nc.psum_tensor(f"p2_{l}_{name}", list(shape), dt)).ap()

            qn = sb("qn", [128, 4, S], BF16)
            kn = sb("kn", [128, 4, S], BF16)
            vE = sb("vE", [128, NT, 520], BF16)
            vO = sb("vO", [128, NT, 520], BF16)
            tz = sb("tz", [128, 112, 64], F32)
            cmask = sb("cmask", [128, 64], F32)
            tb = sb("tb", [128, 8, 14, 64], BF16)
            P.ld("sp", tz, tz_in[l].rearrange("p (a q) -> p a q", q=64), [], ["tz"])
            P.ld("sp", cmask, colmask_in, [], ["cmask"])
            P.stt(tb.rearrange("p h d q -> p (h d) q"), tz, 8.0, cmask.unsqueeze(1).broadcast_to([128, 112, 64]),
                  ALU.mult, ALU.add, ["tz", "cmask"], ["tb"])
            pT = [sb(f"pT{i}", [128, 512], BF16) for i in range(2)]
            rec = sb("rec", [64, 8], F32)
            otok = sb("otok", [64, 8, 64], BF16)
            nast = sb("nast", [128, 4, 512], BF16)
            pss = [psb(f"pss{i}") for i in range(3)]
            pso = [psb(f"pso{i}") for i in range(2)]
            ptr = [psb(f"ptr{i}", (128, 1024), BF16) for i in range(2)]

            P.ld("sp", qn, qnT.rearrange("(c p) s -> p c s", p=128), [], ["qn"])
            P.ld("sp", kn, knT.rearrange("(c p) s -> p c s", p=128), [], ["kn"])
            P.ld("sp", vE, rows_view(vn_aug, 0, NT), [], ["vE"])
            P.ld("sp", vO[:, 0:NT - 1, :], rows_view(vn_aug, 64, NT - 1), [], ["vO"])
            items = [(r, hp) for r in range(ROWS) for hp in range(4)]
            qpad = [sb(f"qpad{i}", [128, 4, 128], BF16) for i in range(2)]
            for i_ in range(2):
                P.op("pool", lambda e, i_=i_: e.memset(qpad[i_], 0.0), [], [("qpad", i_)])

            def na_qpad(r):
                qp = qpad[r % 2]
                P.cp("pool", qp[0:64, :, 0:64], qn[0:64, :, 64 * r:64 * r + 64], ["qn"], [("qpad", r % 2)])
                P.cp("pool", qp[64:128, :, 64:128], qn[64:128, :, 64 * r:64 * r + 64], ["qn"], [("qpad", r % 2)])

            def na_qk(idx):
                r, hp = items[idx]
                r0 = min(max(r - 4, 0), ROWS - 8)
                kb = 64 * r0
                psS = pss[idx % 3]
                d0 = r0 - r + 7
                P.mm(psS, identb, tb[:, 2 * hp:2 * hp + 2, d0:d0 + 7:2, :].rearrange("p h c q -> p c h q"), True, False,
                     ["tb", "identb"], [("pss", idx % 3)])
                for c in range(4):
                    P.mm(psS[:, c * 128:(c + 1) * 128], kn[:, hp, kb + 128 * c:kb + 128 * c + 128],
                         qpad[r % 2][:, hp, :], False, c == 3, ["kn", ("qpad", r % 2)], [("pss", idx % 3)])
                P.act(pT[idx % 2], psS, AF.Exp, [("pss", idx % 3)], [("pT", idx % 2)], scale=0.125)

            def na_pv(idx):
                r, hp = items[idx]
                r0 = min(max(r - 4, 0), ROWS - 8)
                kb = 64 * r0
                pTb = pT[idx % 2]
                for h2 in range(2):
                    hh = 2 * hp + h2
                    po = pso[hh // 4]
                    for c in range(4):
                        if r0 % 2 == 0:
                            vch = vE[:, kb // 128 + c, hh * 65:(hh + 1) * 65]
                        else:
                            vch = vO[:, (kb - 64) // 128 + c, hh * 65:(hh + 1) * 65]
                        P.mm(po[0:64, (hh % 4) * 65:(hh % 4) * 65 + 65], pTb[:, c * 128 + h2 * 64:c * 128 + h2 * 64 + 64], vch,
                             c == 0, c == 3, [("pT", idx % 2), "vE", "vO"], [("pso", hh // 4)])

            na_qpad(0)
            na_qk(0)
            for r in range(ROWS):
                if r + 1 < ROWS:
                    na_qpad(r + 1)
                for hp in range(4):
                    idx = r * 4 + hp
                    if idx + 1 < len(items):
                        na_qk(idx + 1)
                    na_pv(idx)
                for g in range(2):
                    pov = pso[g][0:64, 0:260].rearrange("p (h d) -> p h d", d=65)
                    P.op("dve", lambda e, pov=pov, g=g: e.reciprocal(out=rec[:, g * 4:(g + 1) * 4].unsqueeze(2),
                                                                    in_=pov[:, :, 64:65]),
                         [("pso", g)], ["rec"])
                    P.tt("dve", otok[:, g * 4:(g + 1) * 4, :], pov[:, :, 0:64],
                         rec[:, g * 4:(g + 1) * 4].unsqueeze(2).broadcast_to([64, 4, 64]), ALU.mult,
                         [("pso", g), "rec"], ["otok"])
                ptb = ptr[r % 2]
                of = otok.rearrange("p h d -> p (h d)")
                for c in range(4):
                    P.tr(ptb[:, c * 64:(c + 1) * 64], of[:, c * 128:(c + 1) * 128], identb[0:64, 0:64],
                         ["otok", "identb"], [("ptr", r % 2)])
                rr = r % 8
                P.cp("act", nast[:, :, rr * 64:(rr + 1) * 64], ptb[:, 0:256].rearrange("p (c q) -> p c q", c=4),
                     [("ptr", r % 2)], ["nast"])
                if rr == 7:
                    P.ld("sp", blkview(naT, r // 8), nast, ["nast"], ["naT"])
            P.flush()

        with ExitStack() as es:
            def sb(name, shape, dt):
                return es.enter_context(nc.sbuf_tensor(f"p3_{l}_{name}", list(shape), dt)).ap()

            def psb(name, shape=(128, 512), dt=F32):
                return es.enter_context(nc.psum_tensor(f"p3_{l}_{name}", list(shape), dt)).ap()

            vm = sb("vm", [128, NT, 584], BF16)
            kT = [sb(f"kT{i}", [128, S], BF16) for i in range(2)]
            qT = [sb(f"qT{i}", [128, S], BF16) for i in range(2)]
            for i_ in range(2):
                P.op("pool", lambda e, i_=i_: e.memset(kT[i_][96:128, :], 0.0), [], [("kT", i_)])
                P.op("pool", lambda e, i_=i_: e.memset(qT[i_][96:128, :], 0.0), [], [("qT", i_)])
            pTm = [sb(f"pTm{i}", [128, 1024], BF16) for i in range(3)]
            rs = [sb(f"rs{i}", [128, 512], F32) for i in range(2)]
            ov = [sb(f"ov{i}", [64, 512], F32) for i in range(2)]
            mst = [sb(f"mst{i}", [64, 512], BF16) for i in range(2)]
            pss = [psb(f"pss{i}", (128, 1024)) for i in range(3)]
            pso = [psb(f"pso{i}") for i in range(1)]
            pbc = [psb(f"pbc{i}") for i in range(1)]
            P.ld("sp", vm, rows_view(vm_aug, 0, NT), [], ["vm"])
            sc = 96.0 ** -0.5
            it = 0
            qi = 0
            pending = []

            def fin1(a, hh, qb):
                P.cp("dve", rs[a][64:65, :], pso[0][64:65, :], [("pso", 0)], [("rs", a)])
                P.cp("dve", ov[a], pso[0][0:64, :], [("pso", 0)], [("ov", a)])
                P.op("dve", lambda e: e.reciprocal(out=rs[a][64:65, :], in_=rs[a][64:65, :]), [("rs", a)], [("rs", a)])

            def fin2(a, hh, qb):
                P.mm(pbc[0][0:64, :], onesf[64:65, 0:64], rs[a][64:65, :], True, True, [("rs", a), "onesf"], [("pbc", 0)])
                P.tt("dve", mst[a], ov[a], pbc[0][0:64, :], ALU.mult, [("ov", a), ("pbc", 0)], [("mst", a)])
                P.ld("sp", mlaT[hh * 64:(hh + 1) * 64, qb * 512:(qb + 1) * 512], mst[a], [("mst", a)], ["mlaT"])

            for hh in range(8):
                b = hh % 2
                P.ld("sp", kT[b][0:64, :], kmT[hh], [], [("kT", b)])
                P.ld("sp", kT[b][64:96, :], kpeT, [], [("kT", b)])
                P.ld("sp", qT[b][0:96, :], qmT[hh], [], [("qT", b)])
                for qb in range(NB):
                    a = qi % 2

                    NP2 = NT // 2

                    def qk(m, it0):
                        i2 = (it0 + m) % 3
                        for u in range(2):
                            kc = 2 * m + u
                            P.mm(pss[i2][:, u * 512:(u + 1) * 512], kT[b][:, kc * 128:(kc + 1) * 128],
                                 qT[b][:, qb * 512:(qb + 1) * 512], True, True, [("kT", b), ("qT", b)], [("pss", i2)])
                        P.act(pTm[i2], pss[i2], AF.Exp, [("pss", i2)], [("pTm", i2)], scale=sc)

                    def pv(m, it0):
                        i2 = (it0 + m) % 3
                        for u in range(2):
                            kc = 2 * m + u
                            P.mm(pso[0], vm[:, kc, hh * 65:hh * 65 + 128], pTm[i2][:, u * 512:(u + 1) * 512],
                                 kc == 0, kc == NT - 1, [("pTm", i2), "vm"], [("pso", 0)])

                    qk(0, it)
                    if NP2 > 1:
                        qk(1, it)
                    for m in range(NP2):
                        if m + 2 < NP2:
                            qk(m + 2, it)
                        pv(m, it)
                        if m == min(2, NP2 - 1) and pending:
                            fin2(*pending.pop())
                    it += NP2
                    fin1(a, hh, qb)
                    pending.append((a, hh, qb))
                    qi += 1
            while pending:
                fin2(*pending.pop())
            P.flush()

        with ExitStack() as es:
            def sb(name, shape, dt):
                return es.enter_context(nc.sbuf_tensor(f"p4_{l}_{name}", list(shape), dt)).ap()

            def psb(name, shape=(128, 512), dt=F32):
                return es.enter_context(nc.psum_tensor(f"p4_{l}_{name}", list(shape), dt)).ap()

            wna = sb("wna", [128, 4, D], BF16)
            wml = sb("wml", [128, 4, D], BF16)
            wo = sb("wo", [128, 8, D], BF16)
            wr = sb("wr", [128, 8, 16], F32)
            gmoe = sb("gmoe", [128, D], F32)
            nab2 = [sb(f"nab{i}", [128, 4, 512], BF16) for i in range(2)]
            mlb2 = [sb(f"mlb{i}", [128, 4, 512], BF16) for i in range(2)]
            gab2 = [sb(f"gab{i}", [128, 8, 512], BF16) for i in range(2)]
            gbb2 = [sb(f"gbb{i}", [128, 8, 512], BF16) for i in range(2)]
            ta = sb("ta", [128, 512], F32)
            tb2 = sb("tb2", [128, 512], F32)
            ta1 = sb("ta1", [128, 512], F32)
            tb21 = sb("tb21", [128, 512], F32)
            mg = sb("mg", [128, 8, 512], BF16)
            xb2 = [sb(f"xblk{i}", [128, 4, D], F32) for i in range(2)]
            junk = sb("junk", [128, D], BF16)
            ss = sb("ss", [128, 4], F32)
            rstd = sb("rstd", [128, 4], F32)
            h2f = sb("h2f", [128, D], F32)
            h2T = sb("h2T", [128, 8, 128], F32)
            rows = sb("rows", [128, 4, RW], BF16)
            lg = sb("lg", [128, 16], F32)
            mx = sb("mx", [128, 1], F32)
            sm = sb("sm", [128, 1], F32)
            ps = [psb(f"ps{i}") for i in range(6)]
            ptf = [psb(f"ptf{i}") for i in range(2)]
            psi = [0]

            def nps():
                i = psi[0] % 6
                psi[0] += 1
                return ps[i], ("ps", i)

            for c in range(4):
                P.ld("pool", wna[:, c, :], w_na_o[l, c * 128:(c + 1) * 128, :], [], ["wna"] if c == 3 else [("wna", c)])
                P.ld("pool", wml[:, c, :], w_mla_o[l, c * 128:(c + 1) * 128, :], [], ["wml"] if c == 3 else [("wml", c)])
            for c in range(8):
                P.ld("pool", wo[:, c, :], w_out[l, c * 128:(c + 1) * 128, :], [], [("wo", c)])
            P.ld("sp", wr, w_router[l].rearrange("(c p) e -> p c e", p=128), [], ["wr"])
            P.ld("sp", gmoe, norm_moe[l].partition_broadcast(128), [], ["gmoe"])
            wna_k = ["wna", ("wna", 0), ("wna", 1), ("wna", 2)]
            wml_k = ["wml", ("wml", 0), ("wml", 1), ("wml", 2)]
            xb3 = [xb2[0], xb2[1], sb("xblk2", [128, 4, D], F32)]
            rows2 = [rows, sb("rows1", [128, 4, RW], BF16)]
            for j_ in range(2):
                P.op("pool", lambda e, j_=j_: e.memset(rows2[j_][:, :, D + 34:RW], 0.0), [],
                     [("rows", j_, t) for t in range(4)])
            h2f2 = [h2f, sb("h2f1", [128, D], F32)]
            ss2 = [ss, sb("ss1", [128, 4], F32)]
            rstd2 = [rstd, sb("rstd1", [128, 4], F32)]

            def p4_load(blk):
                i = blk % 2
                P.ld("sp", nab2[i], blkview(naT, blk), [], [("nab", i)])
                P.ld("sp", mlb2[i], blkview(mlaT, blk), [], [("mlb", i)])
                P.ld("sp", gab2[i], blkview(gaT, blk), [], [("gab", i)])
                P.ld("sp", gbb2[i], blkview(gbT, blk), [], [("gbb", i)])
                P.ld("sp", xb3[blk % 3], rows_view(xsrc, blk * 512, 4), [], [("xblk", blk % 3)])

            def router_pre(blk):
                xblk, xk = xb3[blk % 3], ("xblk", blk % 3)
                ssb, rsb, j = ss2[blk % 2], rstd2[blk % 2], blk % 2
                for t in range(4):
                    P.act(junk, xblk[:, t, :], AF.Square, [xk], ["junk", ("ss", j)], accum_out=ssb[:, t:t + 1])
                P.ts("dve", rsb, ssb, 1.0 / D, EPS, ALU.mult, ALU.add, [("ss", j)], [("rstd", j)])
                P.op("act", lambda e: e.sqrt(out=rsb, in_=rsb), [("rstd", j)], [("rstd", j)])
                P.op("dve", lambda e: e.reciprocal(out=rsb, in_=rsb), [("rstd", j)], [("rstd", j)])

            def router_tile(blk, t):
                xblk, xk = xb3[blk % 3], ("xblk", blk % 3)
                rsb, j = rstd2[blk % 2], blk % 2
                rws = rows2[j]
                hf_, hk = h2f2[t % 2], ("h2f", t % 2)
                gt = blk * 4 + t
                P.stt(hf_, xblk[:, t, :], rsb[:, t:t + 1], gmoe, ALU.mult, ALU.mult, [xk, ("rstd", j), "gmoe"], [hk])
                P.cp("pool", rws[:, t, 0:D], hf_, [hk], [("rows", j, t)])
                P.op("pool", lambda e: e.iota(rws[:, t, D:D + 2].bitcast(I32), pattern=[[0, 1]],
                                              base=gt * 128, channel_multiplier=1), [], [("rows", j, t)])
                for c in range(8):
                    pf = ptf[c // 4]
                    P.tr(pf[:, (c % 4) * 128:(c % 4 + 1) * 128], hf_[:, c * 128:(c + 1) * 128], identf,
                         [hk, "identf"], [("ptf", c // 4)])
                P.cp("act", h2T[:, 0:4, :], ptf[0].rearrange("p (c n) -> p c n", c=4), [("ptf", 0)], ["h2T"])
                P.cp("dve", h2T[:, 4:8, :], ptf[1].rearrange("p (c n) -> p c n", c=4), [("ptf", 1)], ["h2T"])
                pp, pk = nps()
                for c in range(8):
                    P.mm(pp[:, 0:16], h2T[:, c, :], wr[:, c, :], c == 0, c == 7, ["h2T", "wr"], [pk])
                P.op("dve", lambda e: e.tensor_reduce(out=mx, in_=pp[:, 0:16], axis=AX.X, op=ALU.max, negate=True),
                     [pk], ["mx"])
                P.op("act", lambda e: e.activation(out=lg, in_=pp[:, 0:16], func=AF.Exp, bias=mx, scale=1.0,
                                                   accum_out=sm), [pk, "mx"], ["lg", "sm"])
                P.op("dve", lambda e: e.reciprocal(out=sm, in_=sm), ["sm"], ["sm"])
                P.ts("dve", aff[:, gt, :], lg, sm, None, ALU.mult, None, ["lg", "sm"], ["aff"])
                P.cp("pool", rws[:, t, D + 2:D + 34].bitcast(F32), aff[:, gt, :], ["aff"], [("rows", j, t)])

            def router_post(blk):
                j = blk % 2
                P.ld("sp", rows_view(h2rows, blk * 512, 4), rows2[j], [("rows", j, t) for t in range(4)], ["h2rows"])

            p4_load(0)
            for blk in range(NB):
                tok0 = blk * 512
                bi = blk % 2
                nab, mlb, gab, gbb, xblk = nab2[bi], mlb2[bi], gab2[bi], gbb2[bi], xb3[blk % 3]
                xk = ("xblk", blk % 3)
                if blk + 1 < NB:
                    p4_load(blk + 1)
                for oc in range(8):
                    pa, pka = nps()
                    pb_, pkb = nps()
                    for c in range(4):
                        P.mm(pa, wna[:, c, oc * 128:(oc + 1) * 128], nab[:, c, :], c == 0, c == 3, [("nab", bi)] + wna_k, [pka])
                    for c in range(4):
                        P.mm(pb_, wml[:, c, oc * 128:(oc + 1) * 128], mlb[:, c, :], c == 0, c == 3, [("mlb", bi)] + wml_k, [pkb])
                    ta_, tb_ = (ta, tb2) if oc % 2 == 0 else (ta1, tb21)
                    P.tt("dve", ta_, pa, gab[:, oc, :], ALU.mult, [pka, ("gab", bi)], [("ta", oc % 2)])
                    P.tt("dve", tb_, pb_, gbb[:, oc, :], ALU.mult, [pkb, ("gbb", bi)], [("tb2", oc % 2)])
                    P.tt("pool", mg[:, oc, :], ta_, tb_, ALU.add, [("ta", oc % 2), ("tb2", oc % 2)], [("mg", oc)])
                    if blk > 0 and oc % 2 == 1:
                        router_tile(blk - 1, oc // 2)
                if blk > 0:
                    router_post(blk - 1)
                for t in range(4):
                    for hf in range(2):
                        pp, pk = nps()
                        for c in range(8):
                            P.mm(pp, mg[:, c, t * 128:(t + 1) * 128], wo[:, c, hf * 512:(hf + 1) * 512], c == 0, c == 7,
                                 [("mg", c), ("wo", c)], [pk])
                        P.tt("dve", xblk[:, t, hf * 512:(hf + 1) * 512], pp, xblk[:, t, hf * 512:(hf + 1) * 512], ALU.add,
                             [pk, xk], [xk])
                P.ld("sp", rows_view(xs, tok0, 4), xblk, [xk], ["xs"])
                router_pre(blk)
            for t in range(4):
                router_tile(NB - 1, t)
            router_post(NB - 1)

            P.flush()

        with ExitStack() as es:
            def sb(name, shape, dt):
                return es.enter_context(nc.sbuf_tensor(f"p5_{l}_{name}", list(shape), dt)).ap()

            def psb(name, shape=(128, 512), dt=F32):
                return es.enter_context(nc.psum_tensor(f"p5_{l}_{name}", list(shape), dt)).ap()

            w1 = [sb(f"w1_{i}", [128, 8, D], BF16) for i in range(3)]
            w3 = [sb(f"w3_{i}", [128, 8, D], BF16) for i in range(3)]
            w2 = [sb(f"w2_{i}", [128, 8, D], BF16) for i in range(2)]
            xgs2 = [sb(f"xgs{i}", [128, NG, RW], BF16) for i in range(2)]
            xgT = sb("xgT", [128, 8, CAP], BF16)
            s1 = sb("s1", [128, CAP], F32)
            aT = sb("aT", [128, 8, CAP], BF16)
            ysb = sb("ysb", [128, NG, D], F32)
            vals2 = [sb(f"vals{i}", [128, NG], F32) for i in range(2)]
            ids2 = [sb(f"ids{i}", [128, NG], I32) for i in range(2)]
            idf = sb("idf", [128, NG], F32)
            lst = sb("lst", [128, NG, 5], F32)
            iof = sb("iof", [128, CAP], F32)
            ioi = sb("ioi", [128, CAP], I32)
            oh = [sb(f"oh{i}", [128, CAP], BF16) for i in range(4)]
            Rt = sb("Rt", [128, NT, 16, 5], BF16)
            tpi = sb("tpi", [128, NT, 2], I32)
            ahi = sb("ahi", [128, NT, 16], BF16)
            amid = sb("amid", [128, NT, 16], BF16)
            res1 = sb("res1", [128, NT, 16], F32)
            posf = sb("posf", [128, NT, 16], F32)
            ps = [psb(f"ps{i}") for i in range(5)]
            psl = psb("psl")
            ptr = [psb(f"ptr{i}", (128, 1024), BF16) for i in range(2)]
            psi = [0]

            def nps():
                i = psi[0] % 5
                psi[0] += 1
                return ps[i], ("ps", i)

            def wload13(e_):
                b = e_ % 3
                for c in range(8):
                    P.ld("pool", w1[b][:, c, :], moe_w1[l, e_, c * 128:(c + 1) * 128, :], [], [("w1", b, c)])
                    P.ld("pool", w3[b][:, c, :], moe_w3[l, e_, c * 128:(c + 1) * 128, :], [], [("w3", b, c)])

            def wload2(e_):
                b = e_ % 2
                for c in range(8):
                    P.ld("pool", w2[b][:, c, :], moe_w2[l, e_, c * 128:(c + 1) * 128, :], [], [("w2", b, c)])

            wload13(0)
            wload2(0)
            P.op("pool", lambda e: e.iota(ioi, pattern=[[1, CAP]], base=0, channel_multiplier=0), [], ["ioi"])
            P.cp("dve", iof, ioi, ["ioi"], ["iof"])
            P.op("pool", lambda e: e.iota(tpi[:, :, 0:1], pattern=[[1, NT], [0, 1]], base=0, channel_multiplier=0), [], ["tpi"])
            P.op("pool", lambda e: e.iota(tpi[:, :, 1:2], pattern=[[0, NT], [0, 1]], base=0, channel_multiplier=1), [], ["tpi"])
            lo = sb("lo", [128, 16], F32)
            mid = sb("mid", [128, 16], F32)
            part = sb("part", [128, 16], F32)
            ge = sb("ge", [128, 16], F32)
            NF = NT * 16
            ysbf = ysb.rearrange("p g d -> p (g d)")
            cmpt = ysbf[:, 0:NF].rearrange("p (t e) -> p t e", e=16)
            cA = ysbf[:, NF:2 * NF].rearrange("p (t e) -> p t e", e=16)
            cB = ysbf[:, 2 * NF:3 * NF].rearrange("p (t e) -> p t e", e=16)
            p1 = posf
            P.op("dve", lambda e: e.memset(lo, 0.0), [], ["lo"])
            for k in range(NITER):
                ck = 2.0 ** -(k + 1)
                P.ts("dve", mid, lo, ck, None, ALU.add, None, ["lo"], ["thr"])
                P.tt("dve", cmpt, aff, mid.unsqueeze(1).broadcast_to([128, NT, 16]), ALU.is_ge, ["thr"], ["cmpt"])
                P.op("dve", lambda e: e.tensor_reduce(out=part, in_=cmpt.rearrange("p t e -> p e t"), axis=AX.X, op=ALU.add),
                     ["cmpt"], ["part"])
                pp, pk = nps()
                P.mm(pp[:, 0:16], onesf, part, True, True, ["part", "onesf"], [pk])
                P.ts("dve", ge, pp[:, 0:16], CAP - 0.5, None, ALU.is_ge, None, [pk], ["ge"])
                P.stt(lo, ge, ck, lo, ALU.mult, ALU.add, ["ge", "lo"], ["lo"])
            P.tt("dve", cmpt, aff, lo.unsqueeze(1).broadcast_to([128, NT, 16]), ALU.is_ge, ["lo"], ["cmpt"])
            cm2 = cmpt.rearrange("p t e -> p (t e)")
            pp1, pk1 = nps()
            P.mm(pp1[:, 0:NF], lstr, cm2, True, True, ["cmpt", "lstr"], [pk1])
            pp2, pk2 = nps()
            P.mm(pp2[:, 0:NF], onesf, cm2, True, True, ["cmpt", "onesf"], [pk2])
            P.cp("dve", p1.rearrange("p t e -> p (t e)"), pp1[:, 0:NF], [pk1], ["p1"])
            P.cp("dve", cA.rearrange("p t e -> p (t e)"), pp2[:, 0:NF], [pk2], ["cA"])
            cur, nxt, ck_, nk_ = cA, cB, "cA", "cB"
            s_ = 1
            while s_ < NT:
                P.tt("dve", nxt[:, s_:, :], cur[:, s_:, :], cur[:, 0:NT - s_, :], ALU.add, [ck_], [nk_])
                P.cp("dve", nxt[:, 0:s_, :], cur[:, 0:s_, :], [ck_], [nk_])
                cur, nxt, ck_, nk_ = nxt, cur, nk_, ck_
                s_ *= 2
            P.tt("dve", p1, p1, cur, ALU.add, ["p1", ck_], ["p1"])
            P.tt("dve", p1.rearrange("p t e -> p (t e)"), p1.rearrange("p t e -> p (t e)"), pp2[:, 0:NF], ALU.subtract,
                 ["p1", pk2], ["p1"])
            P.ts("dve", cmpt, cmpt, -1.0e6, 1.0e6, ALU.mult, ALU.add, ["cmpt"], ["cmpt"])
            P.tt("dve", p1, p1, cmpt, ALU.add, ["p1", "cmpt"], ["p1"])
            Rf = Rt.rearrange("p t e k -> p (t e) k")
            P.cp("dve", Rt[:, :, :, 0], tpi[:, :, 0:1].broadcast_to([128, NT, 16]), ["tpi"], ["Rt"])
            P.cp("dve", Rt[:, :, :, 1], tpi[:, :, 1:2].broadcast_to([128, NT, 16]), ["tpi"], ["Rt"])
            P.cp("dve", ahi, aff, [], ["ahi"])
            P.tt("dve", res1, aff, ahi, ALU.subtract, ["ahi"], ["res1"])
            P.cp("dve", amid, res1, ["res1"], ["amid"])
            P.tt("dve", res1, res1, amid, ALU.subtract, ["res1", "amid"], ["res1"])
            P.cp("dve", Rt[:, :, :, 2], ahi, ["ahi"], ["Rt"])
            P.cp("dve", Rt[:, :, :, 3], amid, ["amid"], ["Rt"])
            P.cp("dve", Rt[:, :, :, 4], res1, ["res1"], ["Rt"])

            def list_oh(e_, t):
                P.ts("dve", oh[t % 4], iof, p1[:, t, e_:e_ + 1], None, ALU.is_equal, None, ["iof", "p1"], [("oh", t % 4)])

            def list_mm(e_, t):
                o = oh[t % 4]
                for g in range(NG):
                    P.mm(psl[:, g * 8:g * 8 + 5], o[:, g * 128:(g + 1) * 128], Rt[:, t, e_, :], t == 0 and g == 0,
                         t == NT - 1 and g == NG - 1, [("oh", t % 4), "Rt"], ["psl"])

            def list_step(e_, t):
                list_oh(e_, t)
                list_mm(e_, t)

            def list_fin(e_):
                b = e_ % 2
                P.cp("dve", lst, psl[:, 0:NG * 8].rearrange("p (g k) -> p g k", k=8)[:, :, 0:5], ["psl"], ["lst"])
                P.stt(idf, lst[:, :, 0], 128.0, lst[:, :, 1], ALU.mult, ALU.add, ["lst"], ["idf"])
                P.cp("dve", ids2[b], idf, ["idf"], [("ids", b)])
                P.tt("dve", vals2[b], lst[:, :, 2], lst[:, :, 3], ALU.add, ["lst"], [("vals", b)])
                P.tt("dve", vals2[b], vals2[b], lst[:, :, 4], ALU.add, ["lst", ("vals", b)], [("vals", b)])
                for g in range(NG):
                    P.dma("pool", lambda e, g=g, b=b: e.indirect_dma_start(
                        out=xgs2[b][:, g, :], out_offset=None, in_=h2rows,
                        in_offset=bass.IndirectOffsetOnAxis(ap=ids2[b][:, g:g + 1], axis=0),
                        bounds_check=P.reg(e, S - 1), oob_is_err=False), [("ids", b)], [("xgs", b)])

            def build_lists(e_):
                for t in range(NT):
                    list_step(e_, t)
                list_fin(e_)

            def sadd(e_):
                ids = ids2[e_ % 2]
                for g in range(NG):
                    P.dma("pool", lambda e, g=g, ids=ids: e.indirect_dma_start(
                        out=xs, out_offset=bass.IndirectOffsetOnAxis(ap=ids[:, g:g + 1], axis=0),
                        in_=ysb[:, g, :], in_offset=None, bounds_check=P.reg(e, S - 1), oob_is_err=True, compute_op=ALU.add),
                        [("ysb", g), ("ids", e_ % 2)], ["xs"])

            build_lists(0)
            wload13(1)
            wload2(1)
            wload13(2)
            tri = 0
            for e_ in range(16):
                b = e_ % 2
                xgs, vals = xgs2[b], vals2[b]
                for g in range(NG):
                    pt = ptr[tri % 2]
                    for c in range(8):
                        P.tr(pt[:, c * 128:(c + 1) * 128], xgs[:, g, c * 128:(c + 1) * 128], identb, [("xgs", b), "identb"],
                             [("ptr", tri % 2)])
                    P.cp("act" if g % 2 else "dve", xgT[:, :, g * 128:(g + 1) * 128], pt.rearrange("p (c n) -> p c n", c=8),
                         [("ptr", tri % 2)], ["xgT"])
                    tri += 1
                if e_ > 0:
                    sadd(e_ - 1)
                for fc in range(8):
                    if e_ + 1 < 16:
                        for t in range(fc * NT // 8, (fc + 1) * NT // 8):
                            list_oh(e_ + 1, t)
                    pa, pka = nps()
                    pb_, pkb = nps()
                    for c in range(8):
                        P.mm(pa[:, 0:CAP], w1[e_ % 3][:, c, fc * 128:(fc + 1) * 128], xgT[:, c, :], c == 0, c == 7,
                             ["xgT", ("w1", e_ % 3, c)], [pka])
                    for c in range(8):
                        P.mm(pb_[:, 0:CAP], w3[e_ % 3][:, c, fc * 128:(fc + 1) * 128], xgT[:, c, :], c == 0, c == 7,
                             ["xgT", ("w3", e_ % 3, c)], [pkb])
                    P.act(s1, pa[:, 0:CAP], AF.Silu, [pka], ["s1"])
                    P.tt("dve", aT[:, fc, :], s1, pb_[:, 0:CAP], ALU.mult, ["s1", pkb], ["aT"])
                    if e_ + 1 < 16:
                        for t in range(fc * NT // 8, (fc + 1) * NT // 8):
                            list_mm(e_ + 1, t)
                if e_ + 1 < 16:
                    list_fin(e_ + 1)
                if e_ + 3 < 16:
                    wload13(e_ + 3)
                for g in range(NG):
                    for hf in range(2):
                        pp, pk = nps()
                        for c in range(8):
                            P.mm(pp, aT[:, c, g * 128:(g + 1) * 128], w2[b][:, c, hf * 512:(hf + 1) * 512], c == 0, c == 7,
                                 ["aT", ("w2", b, c)], [pk])
                        P.ts("dve", ysb[:, g, hf * 512:(hf + 1) * 512], pp, vals[:, g:g + 1], None, ALU.mult, None,
                             [pk, ("vals", b), "p1"], [("ysb", g)])
                if e_ + 2 < 16:
                    wload2(e_ + 2)
            sadd(15)
            P.flush()

        with ExitStack() as es:
            def sb(name, shape, dt):
                return es.enter_context(nc.sbuf_tensor(f"p6_{l}_{name}", list(shape), dt)).ap()

            def psb(name, shape=(128, 512), dt=F32):
                return es.enter_context(nc.psum_tensor(f"p6_{l}_{name}", list(shape), dt)).ap()

            wg = sb("wg", [128, 8, D], BF16)
            wp = sb("wp", [128, 2, D], BF16)
            gple = sb("gple", [128, D], F32)
            gfin = sb("gfin", [128, D], F32)
            xb2 = [sb(f"xblk{i}", [128, 4, D], F32) for i in range(2)]
            pb2 = [sb(f"pblk{i}", [128, 4, 256], F32) for i in range(2)]
            pbf2 = [sb(f"pbf{i}", [128, 4, 256], BF16) for i in range(2)]
            junk = sb("junk", [128, D], BF16)
            ss2 = [sb(f"ss{i}", [128, 4], F32) for i in range(2)]
            rstd2 = [sb(f"rstd{i}", [128, 4], F32) for i in range(2)]
            ssf = sb("ssf", [128, 4], F32)
            rstdf = sb("rstdf", [128, 4], F32)
            h2b = [sb(f"h{i}", [128, 4, D], BF16) for i in range(2)]
            hT = sb("hT", [128, 8, 512], BF16)
            pT = sb("pT", [128, 2, 512], BF16)
            sg = sb("sg", [128, 512], F32)
            sg1 = sb("sg1", [128, 512], F32)
            yb = sb("yb", [128, 4, D], F32)
            ps = [psb(f"ps{i}") for i in range(6)]
            pst = [psb(f"pst{i}", (128, 1024), BF16) for i in range(2)]
            psi = [0]

            def nps():
                i = psi[0] % 6
                psi[0] += 1
                return ps[i], ("ps", i)

            for c in range(8):
                P.ld("pool", wg[:, c, :], ple_gate_w[l, c * 128:(c + 1) * 128, :], [], [("wg", c)])
            for c in range(2):
                P.ld("pool", wp[:, c, :], ple_w[l, c * 128:(c + 1) * 128, :], [], [("wp", c)])
            P.ld("sp", gple, norm_ple[l].partition_broadcast(128), [], ["gple"])
            P.ld("sp", gfin, norm_final[0].partition_broadcast(128), [], ["gfin"])
            last = (l == L - 1)

            def pro_a(blk):
                i = blk % 2
                tok0 = blk * 512
                xblk, pblk, pbf, ss, rstd, h = xb2[i], pb2[i], pbf2[i], ss2[i], rstd2[i], h2b[i]
                P.ld("sp", xblk, rows_view(xs, tok0, 4), [], [("xblk", i)])
                P.ld("sp", pblk, rows_view(p_in[l], tok0, 4), [], [("pblk", i)])
                for t in range(4):
                    P.act(junk, xblk[:, t, :], AF.Square, [("xblk", i)], ["junk", ("ss", i)], accum_out=ss[:, t:t + 1])
                P.ts("dve", rstd, ss, 1.0 / D, EPS, ALU.mult, ALU.add, [("ss", i)], [("rstd", i)])
                P.op("act", lambda e: e.sqrt(out=rstd, in_=rstd), [("rstd", i)], [("rstd", i)])
                P.op("dve", lambda e: e.reciprocal(out=rstd, in_=rstd), [("rstd", i)], [("rstd", i)])
                P.cp("pool", pbf, pblk, [("pblk", i)], [("pbf", i)])
                for t in range(4):
                    P.stt(h[:, t, :], xblk[:, t, :], rstd[:, t:t + 1], gple, ALU.mult, ALU.mult,
                          [("xblk", i), ("rstd", i), "gple"], [("h", i, t)])

            def pro_b(blk):
                i = blk % 2
                h, pbf = h2b[i], pbf2[i]
                for t in range(4):
                    pt = pst[t % 2]
                    for c in range(8):
                        P.tr(pt[:, c * 128:(c + 1) * 128], h[:, t, c * 128:(c + 1) * 128], identb,
                             [("h", i, t), "identb"], [("pst", t % 2)])
                    P.cp("act" if t % 2 else "dve", hT[:, :, t * 128:(t + 1) * 128],
                         pt.rearrange("p (c n) -> p c n", c=8), [("pst", t % 2)], ["hT"])
                for t in range(4):
                    pt = pst[t % 2]
                    for c in range(2):
                        P.tr(pt[:, c * 128:(c + 1) * 128], pbf[:, t, c * 128:(c + 1) * 128], identb,
                             [("pbf", i), "identb"], [("pst", t % 2)])
                    P.cp("act" if t % 2 else "dve", pT[:, :, t * 128:(t + 1) * 128],
                         pt[:, 0:256].rearrange("p (c n) -> p c n", c=2), [("pst", t % 2)], ["pT"])

            pro_a(0)
            pro_b(0)
            for blk in range(NB):
                tok0 = blk * 512
                bi = blk % 2
                xblk = xb2[bi]
                xk = ("xblk", bi)
                if blk + 1 < NB:
                    pro_a(blk + 1)
                for t in range(4):
                    for hf in range(2):
                        pg, pkg = nps()
                        pl, pkl = nps()
                        for c in range(8):
                            P.mm(pg, hT[:, c, t * 128:(t + 1) * 128], wg[:, c, hf * 512:(hf + 1) * 512], c == 0, c == 7,
                                 ["hT", ("wg", c)], [pkg])
                        for c in range(2):
                            P.mm(pl, pT[:, c, t * 128:(t + 1) * 128], wp[:, c, hf * 512:(hf + 1) * 512], c == 0, c == 1,
                                 ["pT", ("wp", c)], [pkl])
                        sg_ = sg if hf == 0 else sg1
                        sk = ("sg", hf)
                        P.act(sg_, pg, AF.Sigmoid, [pkg], [sk])
                        P.tt("dve", sg_, sg_, pl, ALU.mult, [sk, pkl], [sk])
                        P.tt("pool", xblk[:, t, hf * 512:(hf + 1) * 512], xblk[:, t, hf * 512:(hf + 1) * 512], sg_, ALU.add,
                             [sk, xk], [xk])
                if blk + 1 < NB:
                    pro_b(blk + 1)
                if not last:
                    P.ld("sp", rows_view(xs, tok0, 4), xblk, [xk], ["xs"])
                else:
                    for t in range(4):
                        P.act(junk, xblk[:, t, :], AF.Square, [xk], ["junk", "ssf"], accum_out=ssf[:, t:t + 1])
                    P.ts("dve", rstdf, ssf, 1.0 / D, EPS, ALU.mult, ALU.add, ["ssf"], ["rstdf"])
                    P.op("act", lambda e: e.sqrt(out=rstdf, in_=rstdf), ["rstdf"], ["rstdf"])
                    P.op("dve", lambda e: e.reciprocal(out=rstdf, in_=rstdf), ["rstdf"], ["rstdf"])
                    for t in range(4):
                        P.stt(yb[:, t, :], xblk[:, t, :], rstdf[:, t:t + 1], gfin, ALU.mult, ALU.mult,
                              [xk, "rstdf", "gfin"], ["yb"])
                    P.ld("sp", rows_view(y_out, tok0, 4), yb, ["yb"], ["y"])
            P.flush()
    return nc


def _host_layout(inputs, S, L):
    f = np.float32
    w_in = np.asarray(inputs["w_in"], f)[:L]
    kr = 2176
    swap = np.concatenate([np.arange(8, 16), np.arange(0, 8), np.arange(24, 32), np.arange(16, 24)])
    w_in_ext = np.concatenate([w_in, w_in[:, :, 2112:2176], w_in[:, :, kr + swap]], axis=2)
    wq = np.asarray(inputs["mla_wq_up"], f)[:L].reshape(L, 384, 8, 96)
    wq_ext = np.concatenate([wq, wq[..., 0:64], wq[..., 64 + swap]], axis=3).reshape(L, 384, 8 * 192)
    wkv = np.asarray(inputs["mla_wkv_up"], f)[:L].reshape(L, 256, 8, 128)
    wkv_re = np.concatenate([wkv[..., 0:64].reshape(L, 256, 512), wkv[..., 64:128].reshape(L, 256, 512)], axis=2)
    rpb = np.asarray(inputs["na_rpb"], f)[:L]
    kc = np.arange(64)[:, None]
    qc = np.arange(64)[None, :]
    dc = np.clip(kc - qc + 15, 0, 30)
    tz = np.empty((L, 128, 8, 14, 64), f)
    for krel in range(2):
        for d in range(14):
            tz[:, krel * 64:(krel + 1) * 64, :, d, :] = np.transpose(rpb[:, :, d + krel, :][:, :, dc], (0, 2, 1, 3))
    col_start = np.clip(np.arange(64) - 8, 0, 48)[None, :]
    valid = (kc >= col_start) & (kc < col_start + 16)
    colmask = np.where(valid, 0.0, MASKV).astype(f)
    colmask = np.concatenate([colmask, colmask], axis=0)
    t = np.arange(S)
    freqs = (1.0 / (10000.0 ** (np.arange(0, 16, 2, dtype=f) / f(16)))).astype(f)
    cos = np.empty((32, S), f)
    sin = np.empty((32, S), f)
    for j in range(32):
        pos = (t // 64) if j < 16 else (t % 64)
        jj = j % 16
        ang = pos.astype(f) * freqs[jj % 8]
        cos[j] = np.cos(ang)
        sin[j] = np.sin(ang) * (-1.0 if jj < 8 else 1.0)
    rope = np.stack([cos, sin]).astype(f)
    shared = {
        "norm_mix": inputs["norm_mix"][:L], "w_in_ext": w_in_ext, "tz": tz.reshape(L, 128, -1), "colmask": colmask,
        "rope": rope, "mla_q_norm": inputs["mla_q_norm"][:L], "wq_ext": wq_ext, "mla_kv_norm": inputs["mla_kv_norm"][:L],
        "wkv_re": wkv_re, "w_na_o": inputs["w_na_o"][:L], "w_mla_o": inputs["w_mla_o"][:L], "w_out": inputs["w_out"][:L],
        "norm_moe": inputs["norm_moe"][:L], "w_router": inputs["w_router"][:L], "moe_w1": inputs["moe_w1"][:L],
        "moe_w3": inputs["moe_w3"][:L], "moe_w2": inputs["moe_w2"][:L], "norm_ple": inputs["norm_ple"][:L],
        "ple_gate_w": inputs["ple_gate_w"][:L], "ple_w": inputs["ple_w"][:L],
        "norm_final": np.asarray(inputs["norm_final"], f).reshape(1, D),
    }
    return {k: np.ascontiguousarray(np.asarray(v, f)) for k, v in shared.items()}


def kernel(**inputs):
    x = np.asarray(inputs["x"], np.float32)
    p = np.asarray(inputs["p"], np.float32)
    B, S, _ = x.shape
    L = p.shape[0]
    nc = build(S, L)
    shared = _host_layout(inputs, S, L)
    in_maps = []
    for b in range(B):
        m = dict(shared)
        m["x"] = np.ascontiguousarray(x[b])
        m["p"] = np.ascontiguousarray(p[:, b])
        in_maps.append(m)
    res = run_bass_kernel_spmd(nc, in_maps, core_ids=list(range(B)))
    return np.stack([np.asarray(r["y"], np.float32) for r in res.results], axis=0)
```

```python
from contextlib import ExitStack
import numpy as np
import concourse.bass as bass
import concourse.mybir as mybir
from concourse.bass_utils import run_bass_kernel_spmd

F32 = mybir.dt.float32
BF16 = mybir.dt.bfloat16
I32 = mybir.dt.int32
AF = mybir.ActivationFunctionType
ALU = mybir.AluOpType
AX = mybir.AxisListType

D = 1024
NDMA_SEM = 24
EPS = 1e-6
MASKV = -240000.0


class Prog:
    ENGS = ("pe", "dve", "act", "pool", "sp")

    def __init__(self, nc):
        self.nc = nc
        self.stream = {e: [] for e in self.ENGS}
        self.cnt = {e: 0 for e in self.ENGS}
        self.sem = {}
        self.known = {e: {} for e in self.ENGS}
        self.last_w = {}
        self.readers = {}
        self.dq = {}
        self._ctx = []
        for e in ("pe", "dve", "act", "pool"):
            self.sem[e] = self._mksem("c_" + e)
        for q in ("sp", "pool", "act"):
            self.dq[q] = {"sems": [self._mksem(f"d_{q}{i}") for i in range(NDMA_SEM)], "n": 0}
        self.nops = 0

    def _mksem(self, name):
        cm = self.nc.semaphore(name)
        s = cm.__enter__()
        self._ctx.append(cm)
        return s

    def _need(self, eng, ev):
        sem, val, src, is_dma = ev
        if (not is_dma) and src == eng and eng == "pe":
            return
        k = self.known[eng]
        if k.get(id(sem), 0) >= val:
            return
        k[id(sem)] = val
        self.stream[eng].append(("wait", sem, val))

    def _deps(self, eng, reads, writes):
        for key in reads:
            ev = self.last_w.get(key)
            if ev is not None:
                self._need(eng, ev)
        for key in writes:
            ev = self.last_w.get(key)
            if ev is not None:
                self._need(eng, ev)
            for ev in self.readers.get(key, ()):
                self._need(eng, ev)

    def _commit(self, ev, reads, writes):
        for key in reads:
            self.readers.setdefault(key, []).append(ev)
        for key in writes:
            self.last_w[key] = ev
            self.readers[key] = []

    def op(self, eng, fn, reads=(), writes=()):
        self._deps(eng, reads, writes)
        self.cnt[eng] += 1
        ev = (self.sem[eng], self.cnt[eng], eng, False)
        self.stream[eng].append(("op", fn, self.sem[eng], 1))
        self._commit(ev, reads, writes)
        self.nops += 1
        return ev

    def dma(self, q, fn, reads=(), writes=()):
        d = self.dq[q]
        i = d["n"]
        d["n"] += 1
        sem = d["sems"][i % NDMA_SEM]
        rnd = i // NDMA_SEM
        if rnd > 0:
            self._need(q, (sem, 16 * rnd, q, True))
        self._deps(q, reads, writes)
        ev = (sem, 16 * (rnd + 1), q, True)
        self.stream[q].append(("op", fn, sem, 16))
        self._commit(ev, reads, writes)
        self.nops += 1
        return ev

    def barrier(self):
        for e in self.ENGS:
            for e2 in ("pe", "dve", "act", "pool"):
                if self.cnt[e2] > 0:
                    self._need(e, (self.sem[e2], self.cnt[e2], e2, False))
            for q, d in self.dq.items():
                for j, sem in enumerate(d["sems"]):
                    n = (d["n"] - j + NDMA_SEM - 1) // NDMA_SEM if d["n"] > j else 0
                    if n > 0:
                        self._need(e, (sem, 16 * n, q, True))
        self.last_w = {}
        self.readers = {}

    def flush(self):
        self.barrier()
        nc = self.nc
        emap = {"pe": "tensor", "dve": "vector", "act": "scalar", "pool": "gpsimd", "sp": "sync"}
        with nc.Block() as block:
            for e in self.ENGS:
                items = self.stream[e]

                def body(eng, items=items):
                    self.regcache = {}
                    for it in items:
                        if it[0] == "wait":
                            eng.wait_ge(it[1], it[2])
                        else:
                            it[1](eng).then_inc(it[2], it[3])

                getattr(block, emap[e])(body)
        self.stream = {e: [] for e in self.ENGS}

    def reg(self, eng, val):
        if val not in self.regcache:
            self.regcache[val] = eng.to_reg(val)
        return self.regcache[val]

    def mm(self, out, lhsT, rhs, start, stop, reads, writes):
        return self.op("pe", lambda e: e.matmul(out, lhsT=lhsT, rhs=rhs, start=start, stop=stop), reads, writes)

    def tr(self, out, in_, ident, reads, writes):
        return self.op("pe", lambda e: e.transpose(out=out, in_=in_, identity=ident), reads, writes)

    def act(self, out, in_, func, reads, writes, scale=1.0, accum_out=None):
        if accum_out is None:
            return self.op("act", lambda e: e.activation(out=out, in_=in_, func=func, scale=scale), reads, writes)
        return self.op("act", lambda e: e.activation(out=out, in_=in_, func=func, scale=scale, accum_out=accum_out),
                       reads, writes)

    def tt(self, eng, out, in0, in1, op, reads, writes):
        return self.op(eng, lambda e: e.tensor_tensor(out=out, in0=in0, in1=in1, op=op), reads, writes)

    def ts(self, eng, out, in0, s1, s2, op0, op1, reads, writes):
        if op1 is None:
            return self.op(eng, lambda e: e.tensor_scalar(out=out, in0=in0, scalar1=s1, scalar2=None, op0=op0),
                           reads, writes)
        return self.op(eng, lambda e: e.tensor_scalar(out=out, in0=in0, scalar1=s1, scalar2=s2, op0=op0, op1=op1),
                       reads, writes)

    def stt(self, out, in0, scalar, in1, op0, op1, reads, writes):
        return self.op("dve", lambda e: e.scalar_tensor_tensor(out=out, in0=in0, scalar=scalar, in1=in1, op0=op0, op1=op1),
                       reads, writes)

    def cp(self, eng, out, in_, reads, writes):
        if eng == "act":
            return self.op("act", lambda e: e.copy(out=out, in_=in_), reads, writes)
        return self.op(eng, lambda e: e.tensor_copy(out=out, in_=in_), reads, writes)

    def ld(self, q, out, in_, reads, writes):
        return self.dma(q, lambda e: e.dma_start(out=out, in_=in_), reads, writes)


def build(S=4096, L=2, dbg=()):
    NT = S // 128
    NB = S // 512
    ROWS = S // 64
    CAP = 2 * S // 16
    NG = CAP // 128
    NITER = 30
    nc = bass.Bass("TRN2", target_bir_lowering=False)
    P = Prog(nc)

    def din(name, shape, dt=F32):
        return nc.dram_tensor(name, list(shape), dt, kind="ExternalInput").ap()

    def dscr(name, shape, dt):
        return nc.dram_tensor(name, list(shape), dt).ap()

    x_in = din("x", [S, D])
    p_in = din("p", [L, S, 256])
    norm_mix = din("norm_mix", [L, D])
    w_in = din("w_in_ext", [L, D, 4352])
    tz_in = din("tz", [L, 128, 8 * 14 * 64])
    colmask_in = din("colmask", [128, 64])
    rope_in = din("rope", [2, 32, S])
    q_norm = din("mla_q_norm", [L, 384])
    wq_in = din("wq_ext", [L, 384, 8 * 192])
    kv_norm = din("mla_kv_norm", [L, 256])
    wkv_in = din("wkv_re", [L, 256, 1024])
    w_na_o = din("w_na_o", [L, 512, D])
    w_mla_o = din("w_mla_o", [L, 512, D])
    w_out = din("w_out", [L, D, D])
    norm_moe = din("norm_moe", [L, D])
    w_router = din("w_router", [L, D, 16])
    moe_w1 = din("moe_w1", [L, 16, D, D])
    moe_w3 = din("moe_w3", [L, 16, D, D])
    moe_w2 = din("moe_w2", [L, 16, D, D])
    norm_ple = din("norm_ple", [L, D])
    ple_gate_w = din("ple_gate_w", [L, D, D])
    ple_w = din("ple_w", [L, 256, D])
    norm_final = din("norm_final", [1, D])
    y_out = nc.dram_tensor("y", [S, D], F32, kind="ExternalOutput").ap()

    xs = dscr("xs", [S, D], F32)
    qnT = dscr("qnT", [512, S], BF16)
    knT = dscr("knT", [512, S], BF16)
    vn_aug = dscr("vn_aug", [S, 520], BF16)
    qmT = dscr("qmT", [8, 96, S], BF16)
    kmT = dscr("kmT", [8, 64, S], BF16)
    kpeT = dscr("kpeT", [32, S], BF16)
    vm_aug = dscr("vm_aug", [S, 584], BF16)
    gaT = dscr("gaT", [D, S], BF16)
    gbT = dscr("gbT", [D, S], BF16)
    naT = dscr("naT", [512, S], BF16)
    mlaT = dscr("mlaT", [512, S], BF16)
    RW = 1152
    h2rows = dscr("h2rows", [S, RW], BF16)
    xg = [dscr(f"xg{e}", [CAP, RW], BF16) for e in range(16)]
    dbg_t = {}
    for name, shape, dt in dbg:
        dbg_t[name] = nc.dram_tensor("dbg_" + name, list(shape), dt, kind="ExternalOutput").ap()

    def palloc(name, shape, dt):
        return nc.alloc_sbuf_tensor(name, list(shape), dt).ap()

    identf = palloc("identf", [128, 128], F32)
    identb = palloc("identb", [128, 128], BF16)
    onesf = palloc("onesf", [128, 128], F32)
    lstr = palloc("lstr", [128, 128], F32)
    posi = palloc("posi", [128, NT, 16], I32)
    aff = palloc("aff", [128, NT, 16], F32)
    P.op("pool", lambda e: e.memset(identf, 0.0), writes=["identf"])
    P.op("pool", lambda e: e.affine_select(out=identf, in_=identf, pattern=[[-1, 128]], compare_op=ALU.not_equal,
                                           fill=1.0, base=0, channel_multiplier=1), ["identf"], ["identf"])
    P.cp("dve", identb, identf, ["identf"], ["identb"])
    P.op("pool", lambda e: e.memset(onesf, 1.0), writes=["onesf"])
    P.op("pool", lambda e: e.memset(lstr, 1.0), writes=["lstr"])
    P.op("pool", lambda e: e.affine_select(out=lstr, in_=lstr, pattern=[[1, 128]], compare_op=ALU.is_gt,
                                           fill=0.0, base=0, channel_multiplier=-1), ["lstr"], ["lstr"])
    P.flush()

    def blkview(ap2d, blk):
        return ap2d.rearrange("(c p) s -> p c s", p=128)[:, :, blk * 512:(blk + 1) * 512]

    def rows_view(ap2d, r0, nt):
        return ap2d[r0:r0 + 128 * nt, :].rearrange("(t p) f -> p t f", p=128)

    hT_d = dscr("hT_d", [D, S], BF16)

    for l in range(L):
        xsrc = x_in if l == 0 else xs

        with ExitStack() as es:
            def sb(name, shape, dt):
                return es.enter_context(nc.sbuf_tensor(f"p1_{l}_{name}", list(shape), dt)).ap()

            def psb(name, shape=(128, 512), dt=F32):
                return es.enter_context(nc.psum_tensor(f"p1_{l}_{name}", list(shape), dt)).ap()

            win = sb("win", [128, 8, 2304], BF16)
            wq = sb("wq", [128, 3, 1536], BF16)
            wkv = sb("wkv", [128, 2, 1024], BF16)
            gmix = sb("gmix", [128, D], F32)
            gq = sb("gq", [128, 3], F32)
            gkv = sb("gkv", [128, 2], F32)
            xb2 = [sb(f"xblk{i}", [128, 4, D], F32) for i in range(2)]
            junk = sb("junk", [128, D], BF16)
            ss2 = [sb(f"ss{i}", [128, 4], F32) for i in range(2)]
            rstd2 = [sb(f"rstd{i}", [128, 4], F32) for i in range(2)]
            h2b = [sb(f"h{i}", [128, 4, D], BF16) for i in range(2)]
            hT = sb("hT", [128, 8, 512], BF16)
            stq = sb("stq", [128, 4, 512], BF16)
            stk = sb("stk", [128, 4, 512], BF16)
            stv = sb("stv", [128, 4, 520], BF16)
            stv2 = sb("stv2", [128, 4, 584], BF16)
            qlat = sb("qlat", [128, 3, 512], F32)
            kvlat = sb("kvlat", [128, 2, 512], F32)
            sq = sb("sq", [128, 3, 512], F32)
            rt = sb("rt", [128, 512], F32)
            qn_ = sb("qn_", [128, 3, 512], BF16)
            kvn_ = sb("kvn_", [128, 2, 512], BF16)
            cs2 = [sb(f"cs{i}", [128, 2, 512], F32) for i in range(2)]
            t1 = sb("t1", [128, 512], F32)
            t2 = sb("t2", [128, 512], F32)
            kpest = sb("kpest", [128, 512], BF16)
            qst = sb("qst", [128, 8, 512], BF16)
            kst = sb("kst", [128, 8, 512], BF16)
            ps = [psb(f"ps{i}") for i in range(6)]
            pst = [psb(f"pst{i}", (128, 1024), BF16) for i in range(2)]
            psi = [0]

            def nps():
                i = psi[0] % 6
                psi[0] += 1
                return ps[i], ("ps", i)

            for c in range(8):
                rs = slice(c * 128, (c + 1) * 128)
                P.ld("pool", win[:, c, 0:1024], w_in[l, rs, 0:1024], [], [("win", c, 0)])
                P.ld("pool", win[:, c, 1024:2048], w_in[l, rs, 1024:2048], [], [("win", c, 1)])
                P.ld("pool", win[:, c, 2048:2208], w_in[l, rs, 2048:2208], [], [("win", c, 2)])
                P.ld("pool", win[:, c, 2208:2304], w_in[l, rs, 4256:4352], [], [("win", c, 3)])
            for c in range(3):
                for j in range(2):
                    P.ld("pool", wq[:, c, j * 768:(j + 1) * 768], wq_in[l, c * 128:(c + 1) * 128, j * 768:(j + 1) * 768],
                         [], [("wq", c)] if j == 1 else [("wq0", c)])
            for c in range(2):
                P.ld("pool", wkv[:, c, :], wkv_in[l, c * 128:(c + 1) * 128, :], [], [("wkv", c)])
            P.ld("sp", gmix, norm_mix[l].partition_broadcast(128), [], ["gmix"])
            for c in range(3):
                P.ld("sp", gq[:, c:c + 1], q_norm[l, c * 128:(c + 1) * 128].rearrange("(p o) -> p o", o=1), [], ["gq"])
            for c in range(2):
                P.ld("sp", gkv[:, c:c + 1], kv_norm[l, c * 128:(c + 1) * 128].rearrange("(p o) -> p o", o=1), [], ["gkv"])
            P.op("pool", lambda e: e.memset(stv, 1.0), writes=["stv"])
            P.op("pool", lambda e: e.memset(stv2, 1.0), writes=["stv2"])

            def wkeys(c, col0, m):
                f = lambda col: 3 if col >= 2208 else col // 1024
                return list({("win", c, f(col0)), ("win", c, f(col0 + m - 1))})

            def pro_a(blk):
                i = blk % 2
                tok0 = blk * 512
                xblk, cs, ss, rstd, h = xb2[i], cs2[i], ss2[i], rstd2[i], h2b[i]
                P.ld("sp", xblk, rows_view(xsrc, tok0, 4), [], [("xblk", i)])
                P.ld("sp", cs[64:96, 0, :], rope_in[0, :, tok0:tok0 + 512], [], [("cs", i)])
                P.ld("sp", cs[64:96, 1, :], rope_in[1, :, tok0:tok0 + 512], [], [("cs", i)])
                for t in range(4):
                    P.act(junk, xblk[:, t, :], AF.Square, [("xblk", i)], ["junk", ("ss", i)], accum_out=ss[:, t:t + 1])
                P.ts("dve", rstd, ss, 1.0 / D, EPS, ALU.mult, ALU.add, [("ss", i)], [("rstd", i)])
                P.op("act", lambda e: e.sqrt(out=rstd, in_=rstd), [("rstd", i)], [("rstd", i)])
                P.op("dve", lambda e: e.reciprocal(out=rstd, in_=rstd), [("rstd", i)], [("rstd", i)])
                for t in range(4):
                    P.stt(h[:, t, :], xblk[:, t, :], rstd[:, t:t + 1], gmix, ALU.mult, ALU.mult,
                          [("xblk", i), ("rstd", i), "gmix"], [("h", i, t)])

            def pro_b(blk):
                i = blk % 2
                h = h2b[i]
                for t in range(4):
                    pt = pst[t % 2]
                    for c in range(8):
                        P.tr(pt[:, c * 128:(c + 1) * 128], h[:, t, c * 128:(c + 1) * 128], identb,
                             [("h", i, t), "identb"], [("pst", t % 2)])
                    P.cp("act" if t % 2 else "dve", hT[:, :, t * 128:(t + 1) * 128],
                         pt.rearrange("p (c n) -> p c n", c=8), [("pst", t % 2)], ["hT"])
                P.ld("sp", blkview(hT_d, blk), hT, ["hT"], ["hT_d"])

            pro_a(0)
            pro_b(0)
            for blk in range(NB):
                tok0 = blk * 512
                cs = cs2[blk % 2]
                csk = ("cs", blk % 2)
                if blk + 1 < NB:
                    pro_a(blk + 1)

                def fm_group(col0, m):
                    pp, pk = nps()
                    for c in range(8):
                        P.mm(pp[0:m, :], win[:, c, col0:col0 + m], hT[:, c, :], c == 0, c == 7,
                             ["hT"] + wkeys(c, col0, m), [pk])
                    return pp, pk

                def lat_sq(src, nch, tag):
                    for c in range(nch):
                        P.act(sq[:, c, :], src[:, c, :], AF.Square, [tag], ["sq"])

                def lat_fin(src, nch, dim, gvec, gkey, dst, tag):
                    pp, pk = nps()
                    for c in range(nch):
                        P.mm(pp, onesf, sq[:, c, :], c == 0, c == nch - 1, ["sq", "onesf"], [pk])
                    P.ts("dve", rt, pp, 1.0 / dim, EPS, ALU.mult, ALU.add, [pk], ["rt"])
                    P.op("act", lambda e: e.sqrt(out=rt, in_=rt), ["rt"], ["rt"])
                    P.op("dve", lambda e: e.reciprocal(out=rt, in_=rt), ["rt"], ["rt"])
                    for c in range(nch):
                        P.stt(dst[:, c, :], src[:, c, :], gvec[:, c:c + 1], rt, ALU.mult, ALU.mult,
                              [tag, "rt", gkey], [tag + "n"])

                for j in range(3):
                    pp, pk = fm_group(1536 + j * 128, 128)
                    P.cp("act", qlat[:, j, :], pp, [pk], ["qlat"])
                for j in range(2):
                    pp, pk = fm_group(1920 + j * 128, 128)
                    P.cp("dve", kvlat[:, j, :], pp, [pk], ["kvlat"])
                lat_sq(qlat, 3, "qlat")
                for j in range(4):
                    pp, pk = fm_group(j * 128, 128)
                    P.cp("act", stq[:, j, :], pp, [pk], ["stq"])
                P.ld("sp", blkview(qnT, blk), stq, ["stq"], ["qnT"])
                lat_fin(qlat, 3, 384, gq, "gq", qn_, "qlat")
                lat_sq(kvlat, 2, "kvlat")
                for j in range(4):
                    pp, pk = fm_group(512 + j * 128, 128)
                    P.cp("dve", stk[:, j, :], pp, [pk], ["stk"])
                P.ld("sp", blkview(knT, blk), stk, ["stk"], ["knT"])
                lat_fin(kvlat, 2, 256, gkv, "gkv", kvn_, "kvlat")
                for t in range(4):
                    pp, pk = nps()
                    for c in range(8):
                        P.mm(pp, hT[:, c, t * 128:(t + 1) * 128], win[:, c, 1024:1536], c == 0, c == 7,
                             ["hT", ("win", c, 1)], [pk])
                    P.cp("act", stv[:, t, :].rearrange("p (h d) -> p h d", d=65)[:, :, 0:64],
                         pp.rearrange("p (h d) -> p h d", d=64), [pk], ["stv"])
                P.ld("sp", rows_view(vn_aug, tok0, 4), stv, ["stv"], ["vn_aug"])
                ppa, pka = fm_group(2112, 96)
                ppb, pkb = fm_group(2208, 96)
                P.tt("dve", t1[64:96, :], ppa[64:96, :], cs[64:96, 0, :], ALU.mult, [pka, csk], ["t1"])
                P.tt("dve", t2[64:96, :], ppb[64:96, :], cs[64:96, 1, :], ALU.mult, [pkb, csk], ["t2"])
                P.tt("pool", kpest[64:96, :], t1[64:96, :], t2[64:96, :], ALU.add, ["t1", "t2"], ["kpest"])
                P.ld("sp", kpeT[:, tok0:tok0 + 512], kpest[64:96, :], ["kpest"], ["kpeT"])
                if blk + 1 < NB:
                    pro_b(blk + 1)

                for hh in range(8):
                    ppa, pka = nps()
                    ppb, pkb = nps()
                    for c in range(3):
                        P.mm(ppa[0:96, :], wq[:, c, hh * 192:hh * 192 + 96], qn_[:, c, :], c == 0, c == 2,
                             ["qlatn", ("wq", c), ("wq0", c)], [pka])
                    for c in range(3):
                        P.mm(ppb[0:96, :], wq[:, c, hh * 192 + 96:hh * 192 + 192], qn_[:, c, :], c == 0, c == 2,
                             ["qlatn", ("wq", c), ("wq0", c)], [pkb])
                    P.cp("act", qst[0:64, hh, :], ppa[0:64, :], [pka], ["qst"])
                    P.tt("dve", t1[64:96, :], ppa[64:96, :], cs[64:96, 0, :], ALU.mult, [pka, csk], ["t1"])
                    P.tt("dve", t2[64:96, :], ppb[64:96, :], cs[64:96, 1, :], ALU.mult, [pkb, csk], ["t2"])
                    P.tt("pool", qst[64:96, hh, :], t1[64:96, :], t2[64:96, :], ALU.add, ["t1", "t2"], ["qst"])
                P.ld("sp", qmT.rearrange("h r s -> r h s")[:, :, tok0:tok0 + 512], qst[0:96], ["qst"], ["qmT"])
                for hh in range(8):
                    pp, pk = nps()
                    for c in range(2):
                        P.mm(pp[0:64, :], wkv[:, c, hh * 64:(hh + 1) * 64], kvn_[:, c, :], c == 0, c == 1,
                             ["kvlatn", ("wkv", c)], [pk])
                    P.cp("act" if hh % 2 else "dve", kst[0:64, hh, :], pp[0:64, :], [pk], ["kst"])
                P.ld("sp", kmT.rearrange("h r s -> r h s")[:, :, tok0:tok0 + 512], kst[0:64], ["kst"], ["kmT"])
                for t in range(4):
                    pp, pk = nps()
                    for c in range(2):
                        P.mm(pp, kvn_[:, c, t * 128:(t + 1) * 128], wkv[:, c, 512:1024], c == 0, c == 1,
                             ["kvlatn", ("wkv", c)], [pk])
                    P.cp("act", stv2[:, t, 0:520].rearrange("p (h d) -> p h d", d=65)[:, :, 0:64],
                         pp.rearrange("p (h d) -> p h d", d=64), [pk], ["stv2"])
                P.ld("sp", rows_view(vm_aug, tok0, 4), stv2, ["stv2"], ["vm_aug"])
            P.flush()

        with ExitStack() as es:
            def sb(name, shape, dt):
                return es.enter_context(nc.sbuf_tensor(f"p1b_{l}_{name}", list(shape), dt)).ap()

            def psb(name, shape=(128, 512), dt=F32):
                return es.enter_context(nc.psum_tensor(f"p1b_{l}_{name}", list(shape), dt)).ap()

            wing = sb("wing", [128, 8, 2048], BF16)
            hTb = [sb(f"hTb{i}", [128, 8, 512], BF16) for i in range(2)]
            stg = [sb(f"stg{i}", [128, 8, 512], BF16) for i in range(2)]
            ps = [psb(f"ps{i}") for i in range(8)]
            for c in range(8):
                for j in range(2):
                    P.ld("pool", wing[:, c, j * 1024:(j + 1) * 1024],
                         w_in[l, c * 128:(c + 1) * 128, 2208 + j * 1024:2208 + (j + 1) * 1024], [], [("wing", c, j)])
            gi_ = 0
            P.ld("sp", hTb[0], blkview(hT_d, 0), [], [("hTb", 0)])
            for blk in range(NB):
                hb = hTb[blk % 2]
                if blk + 1 < NB:
                    P.ld("sp", hTb[(blk + 1) % 2], blkview(hT_d, blk + 1), [], [("hTb", (blk + 1) % 2)])
                for gi, gdst in enumerate((gaT, gbT)):
                    sg_ = stg[gi]
                    for j in range(8):
                        pp, pk = ps[gi_ % 8], ("ps", gi_ % 8)
                        gi_ += 1
                        col0 = gi * 1024 + j * 128
                        for c in range(8):
                            P.mm(pp, wing[:, c, col0:col0 + 128], hb[:, c, :], c == 0, c == 7,
                                 [("hTb", blk % 2), ("wing", c, gi)], [pk])
                        P.act(sg_[:, j, :], pp, AF.Sigmoid, [pk], [("stg", gi)])
                    P.ld("sp", blkview(gdst, blk), sg_, [("stg", gi)], [("gT", gi)])
            P.flush()

        with ExitStack() as es:
            def sb(name, shape, dt):
                return es.enter_context(nc.sbuf_tensor(f"p2_{l}_{name}", list(shape), dt)).ap()

            def psb(name, shape=(128, 512), dt=F32):
                return es.enter_context(nc.psum_tensor(f"p2_{l}_{name}", list(shape), dt)).ap()

            qn = sb("qn", [128, 4, S], BF16)
            kn = sb("kn", [128, 4, S], BF16)
            vE = sb("vE", [128, NT, 520], BF16)
            vO = sb("vO", [128, NT, 520], BF16)
            tz = sb("tz", [128, 112, 64], F32)
            cmask = sb("cmask", [128, 64], F32)
            tb = sb("tb", [128, 8, 14, 64], BF16)
            P.ld("sp", tz, tz_in[l].rearrange("p (a q) -> p a q", q=64), [], ["tz"])
            P.ld("sp", cmask, colmask_in, [], ["cmask"])
            P.stt(tb.rearrange("p h d q -> p (h d) q"), tz, 8.0, cmask.unsqueeze(1).broadcast_to([128, 112, 64]),
                  ALU.mult, ALU.add, ["tz", "cmask"], ["tb"])
            pT = [sb(f"pT{i}", [128, 512], BF16) for i in range(2)]
            rec = sb("rec", [64, 8], F32)
            otok = sb("otok", [64, 8, 64], BF16)
            nast = sb("nast", [128, 4, 512], BF16)
            pss = [psb(f"pss{i}") for i in range(3)]
            pso = [psb(f"pso{i}") for i in range(2)]
            ptr = [psb(f"ptr{i}", (128, 1024), BF16) for i in range(2)]

            P.ld("sp", qn, qnT.rearrange("(c p) s -> p c s", p=128), [], ["qn"])
            P.ld("sp", kn, knT.rearrange("(c p) s -> p c s", p=128), [], ["kn"])
            P.ld("sp", vE, rows_view(vn_aug, 0, NT), [], ["vE"])
            P.ld("sp", vO[:, 0:NT - 1, :], rows_view(vn_aug, 64, NT - 1), [], ["vO"])
            items = [(r, hp) for r in range(ROWS) for hp in range(4)]
            qpad = [sb(f"qpad{i}", [128, 4, 128], BF16) for i in range(2)]
            for i_ in range(2):
                P.op("pool", lambda e, i_=i_: e.memset(qpad[i_], 0.0), [], [("qpad", i_)])

            def na_qpad(r):
                qp = qpad[r % 2]
                P.cp("pool", qp[0:64, :, 0:64], qn[0:64, :, 64 * r:64 * r + 64], ["qn"], [("qpad", r % 2)])
                P.cp("pool", qp[64:128, :, 64:128], qn[64:128, :, 64 * r:64 * r + 64], ["qn"], [("qpad", r % 2)])

            def na_qk(idx):
                r, hp = items[idx]
                r0 = min(max(r - 4, 0), ROWS - 8)
                kb = 64 * r0
                psS = pss[idx % 3]
                d0 = r0 - r + 7
                P.mm(psS, identb, tb[:, 2 * hp:2 * hp + 2, d0:d0 + 7:2, :].rearrange("p h c q -> p c h q"), True, False,
                     ["tb", "identb"], [("pss", idx % 3)])
                for c in range(4):
                    P.mm(psS[:, c * 128:(c + 1) * 128], kn[:, hp, kb + 128 * c:kb + 128 * c + 128],
                         qpad[r % 2][:, hp, :], False, c == 3, ["kn", ("qpad", r % 2)], [("pss", idx % 3)])
                P.act(pT[idx % 2], psS, AF.Exp, [("pss", idx % 3)], [("pT", idx % 2)], scale=0.125)

            def na_pv(idx):
                r, hp = items[idx]
                r0 = min(max(r - 4, 0), ROWS - 8)
                kb = 64 * r0
                pTb = pT[idx % 2]
                for h2 in range(2):
                    hh = 2 * hp + h2
                    po = pso[hh // 4]
                    for c in range(4):
                        if r0 % 2 == 0:
                            vch = vE[:, kb // 128 + c, hh * 65:(hh + 1) * 65]
                        else:
                            vch = vO[:, (kb - 64) // 128 + c, hh * 65:(hh + 1) * 65]
                        P.mm(po[0:64, (hh % 4) * 65:(hh % 4) * 65 + 65], pTb[:, c * 128 + h2 * 64:c * 128 + h2 * 64 + 64], vch,
                             c == 0, c == 3, [("pT", idx % 2), "vE", "vO"], [("pso", hh // 4)])

            na_qpad(0)
            na_qk(0)
            for r in range(ROWS):
                if r + 1 < ROWS:
                    na_qpad(r + 1)
                for hp in range(4):
                    idx = r * 4 + hp
                    if idx + 1 < len(items):
                        na_qk(idx + 1)
                    na_pv(idx)
                for g in range(2):
                    pov = pso[g][0:64, 0:260].rearrange("p (h d) -> p h d", d=65)
                    P.op("dve", lambda e, pov=pov, g=g: e.reciprocal(out=rec[:, g * 4:(g + 1) * 4].unsqueeze(2),
                                                                    in_=pov[:, :, 64:65]),
                         [("pso", g)], ["rec"])
                    P.tt("dve", otok[:, g * 4:(g + 1) * 4, :], pov[:, :, 0:64],
                         rec[:, g * 4:(g + 1) * 4].unsqueeze(2).broadcast_to([64, 4, 64]), ALU.mult,
                         [("pso", g), "rec"], ["otok"])
                ptb = ptr[r % 2]
                of = otok.rearrange("p h d -> p (h d)")
                for c in range(4):
                    P.tr(ptb[:, c * 64:(c + 1) * 64], of[:, c * 128:(c + 1) * 128], identb[0:64, 0:64],
                         ["otok", "identb"], [("ptr", r % 2)])
                rr = r % 8
                P.cp("act", nast[:, :, rr * 64:(rr + 1) * 64], ptb[:, 0:256].rearrange("p (c q) -> p c q", c=4),
                     [("ptr", r % 2)], ["nast"])
                if rr == 7:
                    P.ld("sp", blkview(naT, r // 8), nast, ["nast"], ["naT"])
            P.flush()

        with ExitStack() as es:
            def sb(name, shape, dt):
                return es.enter_context(nc.sbuf_tensor(f"p3_{l}_{name}", list(shape), dt)).ap()

            def psb(name, shape=(128, 512), dt=F32):
                return es.enter_context(nc.psum_tensor(f"p3_{l}_{name}", list(shape), dt)).ap()

            vm = sb("vm", [128, NT, 584], BF16)
            kT = [sb(f"kT{i}", [128, S], BF16) for i in range(2)]
            qT = [sb(f"qT{i}", [128, S], BF16) for i in range(2)]
            for i_ in range(2):
                P.op("pool", lambda e, i_=i_: e.memset(kT[i_][96:128, :], 0.0), [], [("kT", i_)])
                P.op("pool", lambda e, i_=i_: e.memset(qT[i_][96:128, :], 0.0), [], [("qT", i_)])
            pTm = [sb(f"pTm{i}", [128, 1024], BF16) for i in range(3)]
            rs = [sb(f"rs{i}", [128, 512], F32) for i in range(2)]
            ov = [sb(f"ov{i}", [64, 512], F32) for i in range(2)]
            mst = [sb(f"mst{i}", [64, 512], BF16) for i in range(2)]
            pss = [psb(f"pss{i}", (128, 1024)) for i in range(3)]
            pso = [psb(f"pso{i}") for i in range(1)]
            pbc = [psb(f"pbc{i}") for i in range(1)]
            P.ld("sp", vm, rows_view(vm_aug, 0, NT), [], ["vm"])
            sc = 96.0 ** -0.5
            it = 0
            qi = 0
            pending = []

            def fin1(a, hh, qb):
                P.cp("dve", rs[a][64:65, :], pso[0][64:65, :], [("pso", 0)], [("rs", a)])
                P.cp("dve", ov[a], pso[0][0:64, :], [("pso", 0)], [("ov", a)])
                P.op("dve", lambda e: e.reciprocal(out=rs[a][64:65, :], in_=rs[a][64:65, :]), [("rs", a)], [("rs", a)])

            def fin2(a, hh, qb):
                P.mm(pbc[0][0:64, :], onesf[64:65, 0:64], rs[a][64:65, :], True, True, [("rs", a), "onesf"], [("pbc", 0)])
                P.tt("dve", mst[a], ov[a], pbc[0][0:64, :], ALU.mult, [("ov", a), ("pbc", 0)], [("mst", a)])
                P.ld("sp", mlaT[hh * 64:(hh + 1) * 64, qb * 512:(qb + 1) * 512], mst[a], [("mst", a)], ["mlaT"])

            for hh in range(8):
                b = hh % 2
                P.ld("sp", kT[b][0:64, :], kmT[hh], [], [("kT", b)])
                P.ld("sp", kT[b][64:96, :], kpeT, [], [("kT", b)])
                P.ld("sp", qT[b][0:96, :], qmT[hh], [], [("qT", b)])
                for qb in range(NB):
                    a = qi % 2

                    NP2 = NT // 2

                    def qk(m, it0):
                        i2 = (it0 + m) % 3
                        for u in range(2):
                            kc = 2 * m + u
                            P.mm(pss[i2][:, u * 512:(u + 1) * 512], kT[b][:, kc * 128:(kc + 1) * 128],
                                 qT[b][:, qb * 512:(qb + 1) * 512], True, True, [("kT", b), ("qT", b)], [("pss", i2)])
                        P.act(pTm[i2], pss[i2], AF.Exp, [("pss", i2)], [("pTm", i2)], scale=sc)

                    def pv(m, it0):
                        i2 = (it0 + m) % 3
                        for u in range(2):
                            kc = 2 * m + u
                            P.mm(pso[0], vm[:, kc, hh * 65:hh * 65 + 128], pTm[i2][:, u * 512:(u + 1) * 512],
                                 kc == 0, kc == NT - 1, [("pTm", i2), "vm"], [("pso", 0)])

                    qk(0, it)
                    if NP2 > 1:
                        qk(1, it)
                    for m in range(NP2):
                        if m + 2 < NP2:
                            qk(m + 2, it)
                        pv(m, it)
                        if m == min(2, NP2 - 1) and pending:
                            fin2(*pending.pop())
                    it += NP2
                    fin1(a, hh, qb)
                    pending.append((a, hh, qb))
                    qi += 1
            while pending:
                fin2(*pending.pop())
            P.flush()

        with ExitStack() as es:
            def sb(name, shape, dt):
                return es.enter_context(nc.sbuf_tensor(f"p4_{l}_{name}", list(shape), dt)).ap()

            def psb(name, shape=(128, 512), dt=F32):
                return es.enter_context(nc.psum_tensor(f"p4_{l}_{name}", list(shape), dt)).ap()

            wna = sb("wna", [128, 4, D], BF16)
            wml = sb("wml", [128, 4, D], BF16)
            wo = sb("wo", [128, 8, D], BF16)
            wr = sb("wr", [128, 8, 16], F32)
            gmoe = sb("gmoe", [128, D], F32)
            nab2 = [sb(f"nab{i}", [128, 4, 512], BF16) for i in range(2)]
            mlb2 = [sb(f"mlb{i}", [128, 4, 512], BF16) for i in range(2)]
            gab2 = [sb(f"gab{i}", [128, 8, 512], BF16) for i in range(2)]
            gbb2 = [sb(f"gbb{i}", [128, 8, 512], BF16) for i in range(2)]
            ta = sb("ta", [128, 512], F32)
            tb2 = sb("tb2", [128, 512], F32)
            ta1 = sb("ta1", [128, 512], F32)
            tb21 = sb("tb21", [128, 512], F32)
            mg = sb("mg", [128, 8, 512], BF16)
            xb2 = [sb(f"xblk{i}", [128, 4, D], F32) for i in range(2)]
            junk = sb("junk", [128, D], BF16)
            ss = sb("ss", [128, 4], F32)
            rstd = sb("rstd", [128, 4], F32)
            h2f = sb("h2f", [128, D], F32)
            h2T = sb("h2T", [128, 8, 128], F32)
            rows = sb("rows", [128, 4, RW], BF16)
            lg = sb("lg", [128, 16], F32)
            mx = sb("mx", [128, 1], F32)
            sm = sb("sm", [128, 1], F32)
            ps = [psb(f"ps{i}") for i in range(6)]
            ptf = [psb(f"ptf{i}") for i in range(2)]
            psi = [0]

            def nps():
                i = psi[0] % 6
                psi[0] += 1
                return ps[i], ("ps", i)

            for c in range(4):
                P.ld("pool", wna[:, c, :], w_na_o[l, c * 128:(c + 1) * 128, :], [], ["wna"] if c == 3 else [("wna", c)])
                P.ld("pool", wml[:, c, :], w_mla_o[l, c * 128:(c + 1) * 128, :], [], ["wml"] if c == 3 else [("wml", c)])
            for c in range(8):
                P.ld("pool", wo[:, c, :], w_out[l, c * 128:(c + 1) * 128, :], [], [("wo", c)])
            P.ld("sp", wr, w_router[l].rearrange("(c p) e -> p c e", p=128), [], ["wr"])
            P.ld("sp", gmoe, norm_moe[l].partition_broadcast(128), [], ["gmoe"])
            wna_k = ["wna", ("wna", 0), ("wna", 1), ("wna", 2)]
            wml_k = ["wml", ("wml", 0), ("wml", 1), ("wml", 2)]
            xb3 = [xb2[0], xb2[1], sb("xblk2", [128, 4, D], F32)]
            rows2 = [rows, sb("rows1", [128, 4, RW], BF16)]
            for j_ in range(2):
                P.op("pool", lambda e, j_=j_: e.memset(rows2[j_][:, :, D + 34:RW], 0.0), [],
                     [("rows", j_, t) for t in range(4)])
            h2f2 = [h2f, sb("h2f1", [128, D], F32)]
            ss2 = [ss, sb("ss1", [128, 4], F32)]
            rstd2 = [rstd, sb("rstd1", [128, 4], F32)]

            def p4_load(blk):
                i = blk % 2
                P.ld("sp", nab2[i], blkview(naT, blk), [], [("nab", i)])
                P.ld("sp", mlb2[i], blkview(mlaT, blk), [], [("mlb", i)])
                P.ld("sp", gab2[i], blkview(gaT, blk), [], [("gab", i)])
                P.ld("sp", gbb2[i], blkview(gbT, blk), [], [("gbb", i)])
                P.ld("sp", xb3[blk % 3], rows_view(xsrc, blk * 512, 4), [], [("xblk", blk % 3)])

            def router_pre(blk):
                xblk, xk = xb3[blk % 3], ("xblk", blk % 3)
                ssb, rsb, j = ss2[blk % 2], rstd2[blk % 2], blk % 2
                for t in range(4):
                    P.act(junk, xblk[:, t, :], AF.Square, [xk], ["junk", ("ss", j)], accum_out=ssb[:, t:t + 1])
                P.ts("dve", rsb, ssb, 1.0 / D, EPS, ALU.mult, ALU.add, [("ss", j)], [("rstd", j)])
                P.op("act", lambda e: e.sqrt(out=rsb, in_=rsb), [("rstd", j)], [("rstd", j)])
                P.op("dve", lambda e: e.reciprocal(out=rsb, in_=rsb), [("rstd", j)], [("rstd", j)])

            def router_tile(blk, t):
                xblk, xk = xb3[blk % 3], ("xblk", blk % 3)
                rsb, j = rstd2[blk % 2], blk % 2
                rws = rows2[j]
                hf_, hk = h2f2[t % 2], ("h2f", t % 2)
                gt = blk * 4 + t
                P.stt(hf_, xblk[:, t, :], rsb[:, t:t + 1], gmoe, ALU.mult, ALU.mult, [xk, ("rstd", j), "gmoe"], [hk])
                P.cp("pool", rws[:, t, 0:D], hf_, [hk], [("rows", j, t)])
                P.op("pool", lambda e: e.iota(rws[:, t, D:D + 2].bitcast(I32), pattern=[[0, 1]],
                                              base=gt * 128, channel_multiplier=1), [], [("rows", j, t)])
                for c in range(8):
                    pf = ptf[c // 4]
                    P.tr(pf[:, (c % 4) * 128:(c % 4 + 1) * 128], hf_[:, c * 128:(c + 1) * 128], identf,
                         [hk, "identf"], [("ptf", c // 4)])
                P.cp("act", h2T[:, 0:4, :], ptf[0].rearrange("p (c n) -> p c n", c=4), [("ptf", 0)], ["h2T"])
                P.cp("dve", h2T[:, 4:8, :], ptf[1].rearrange("p (c n) -> p c n", c=4), [("ptf", 1)], ["h2T"])
                pp, pk = nps()
                for c in range(8):
                    P.mm(pp[:, 0:16], h2T[:, c, :], wr[:, c, :], c == 0, c == 7, ["h2T", "wr"], [pk])
                P.op("dve", lambda e: e.tensor_reduce(out=mx, in_=pp[:, 0:16], axis=AX.X, op=ALU.max, negate=True),
                     [pk], ["mx"])
                P.op("act", lambda e: e.activation(out=lg, in_=pp[:, 0:16], func=AF.Exp, bias=mx, scale=1.0,
                                                   accum_out=sm), [pk, "mx"], ["lg", "sm"])
                P.op("dve", lambda e: e.reciprocal(out=sm, in_=sm), ["sm"], ["sm"])
                P.ts("dve", aff[:, gt, :], lg, sm, None, ALU.mult, None, ["lg", "sm"], ["aff"])
                P.cp("pool", rws[:, t, D + 2:D + 34].bitcast(F32), aff[:, gt, :], ["aff"], [("rows", j, t)])

            def router_post(blk):
                j = blk % 2
                P.ld("sp", rows_view(h2rows, blk * 512, 4), rows2[j], [("rows", j, t) for t in range(4)], ["h2rows"])

            p4_load(0)
            for blk in range(NB):
                tok0 = blk * 512
                bi = blk % 2
                nab, mlb, gab, gbb, xblk = nab2[bi], mlb2[bi], gab2[bi], gbb2[bi], xb3[blk % 3]
                xk = ("xblk", blk % 3)
                if blk + 1 < NB:
                    p4_load(blk + 1)
                for oc in range(8):
                    pa, pka = nps()
                    pb_, pkb = nps()
                    for c in range(4):
                        P.mm(pa, wna[:, c, oc * 128:(oc + 1) * 128], nab[:, c, :], c == 0, c == 3, [("nab", bi)] + wna_k, [pka])
                    for c in range(4):
                        P.mm(pb_, wml[:, c, oc * 128:(oc + 1) * 128], mlb[:, c, :], c == 0, c == 3, [("mlb", bi)] + wml_k, [pkb])
                    ta_, tb_ = (ta, tb2) if oc % 2 == 0 else (ta1, tb21)
                    P.tt("dve", ta_, pa, gab[:, oc, :], ALU.mult, [pka, ("gab", bi)], [("ta", oc % 2)])
                    P.tt("dve", tb_, pb_, gbb[:, oc, :], ALU.mult, [pkb, ("gbb", bi)], [("tb2", oc % 2)])
                    P.tt("pool", mg[:, oc, :], ta_, tb_, ALU.add, [("ta", oc % 2), ("tb2", oc % 2)], [("mg", oc)])
                    if blk > 0 and oc % 2 == 1:
                        router_tile(blk - 1, oc // 2)
                if blk > 0:
                    router_post(blk - 1)
                for t in range(4):
                    for hf in range(2):
                        pp, pk = nps()
                        for c in range(8):
                            P.mm(pp, mg[:, c, t * 128:(t + 1) * 128], wo[:, c, hf * 512:(hf + 1) * 512], c == 0, c == 7,
                                 [("mg", c), ("wo", c)], [pk])
                        P.tt("dve", xblk[:, t, hf * 512:(hf + 1) * 512], pp, xblk[:, t, hf * 512:(hf + 1) * 512], ALU.add,
                             [pk, xk], [xk])
                P.ld("sp", rows_view(xs, tok0, 4), xblk, [xk], ["xs"])
                router_pre(blk)
            for t in range(4):
                router_tile(NB - 1, t)
            router_post(NB - 1)

            P.flush()

        with ExitStack() as es:
            def sb(name, shape, dt):
                return es.enter_context(nc.sbuf_tensor(f"p5_{l}_{name}", list(shape), dt)).ap()

            def psb(name, shape=(128, 512), dt=F32):
                return es.enter_context(nc.psum_tensor(f"p5_{l}_{name}", list(shape), dt)).ap()

            w1 = [sb(f"w1_{i}", [128, 8, D], BF16) for i in range(3)]
            w3 = [sb(f"w3_{i}", [128, 8, D], BF16) for i in range(3)]
            w2 = [sb(f"w2_{i}", [128, 8, D], BF16) for i in range(2)]
            xgs2 = [sb(f"xgs{i}", [128, NG, RW], BF16) for i in range(2)]
            xgT = sb("xgT", [128, 8, CAP], BF16)
            s1 = sb("s1", [128, CAP], F32)
            aT = sb("aT", [128, 8, CAP], BF16)
            ysb = sb("ysb", [128, NG, D], F32)
            vals2 = [sb(f"vals{i}", [128, NG], F32) for i in range(2)]
            ids2 = [sb(f"ids{i}", [128, NG], I32) for i in range(2)]
            idf = sb("idf", [128, NG], F32)
            lst = sb("lst", [128, NG, 5], F32)
            iof = sb("iof", [128, CAP], F32)
            ioi = sb("ioi", [128, CAP], I32)
            oh = [sb(f"oh{i}", [128, CAP], BF16) for i in range(4)]
            Rt = sb("Rt", [128, NT, 16, 5], BF16)
            tpi = sb("tpi", [128, NT, 2], I32)
            ahi = sb("ahi", [128, NT, 16], BF16)
            amid = sb("amid", [128, NT, 16], BF16)
            res1 = sb("res1", [128, NT, 16], F32)
            posf = sb("posf", [128, NT, 16], F32)
            ps = [psb(f"ps{i}") for i in range(5)]
            psl = psb("psl")
            ptr = [psb(f"ptr{i}", (128, 1024), BF16) for i in range(2)]
            psi = [0]

            def nps():
                i = psi[0] % 5
                psi[0] += 1
                return ps[i], ("ps", i)

            def wload13(e_):
                b = e_ % 3
                for c in range(8):
                    P.ld("pool", w1[b][:, c, :], moe_w1[l, e_, c * 128:(c + 1) * 128, :], [], [("w1", b, c)])
                    P.ld("pool", w3[b][:, c, :], moe_w3[l, e_, c * 128:(c + 1) * 128, :], [], [("w3", b, c)])

            def wload2(e_):
                b = e_ % 2
                for c in range(8):
                    P.ld("pool", w2[b][:, c, :], moe_w2[l, e_, c * 128:(c + 1) * 128, :], [], [("w2", b, c)])

            wload13(0)
            wload2(0)
            wload13(1)
            wload2(1)
            wload13(2)
            P.op("pool", lambda e: e.iota(ioi, pattern=[[1, CAP]], base=0, channel_multiplier=0), [], ["ioi"])
            P.cp("dve", iof, ioi, ["ioi"], ["iof"])
            P.op("pool", lambda e: e.iota(tpi[:, :, 0:1], pattern=[[1, NT], [0, 1]], base=0, channel_multiplier=0), [], ["tpi"])
            P.op("pool", lambda e: e.iota(tpi[:, :, 1:2], pattern=[[0, NT], [0, 1]], base=0, channel_multiplier=1), [], ["tpi"])
            lo = sb("lo", [128, 16], F32)
            mid = sb("mid", [128, 16], F32)
            part = sb("part", [128, 16], F32)
            ge = sb("ge", [128, 16], F32)
            NF = NT * 16
            ysbf = ysb.rearrange("p g d -> p (g d)")
            cmpt = ysbf[:, 0:NF].rearrange("p (t e) -> p t e", e=16)
            cA = ysbf[:, NF:2 * NF].rearrange("p (t e) -> p t e", e=16)
            cB = ysbf[:, 2 * NF:3 * NF].rearrange("p (t e) -> p t e", e=16)
            p1 = posf
            P.op("dve", lambda e: e.memset(lo, 0.0), [], ["lo"])
            for k in range(NITER):
                ck = 2.0 ** -(k + 1)
                P.ts("dve", mid, lo, ck, None, ALU.add, None, ["lo"], ["thr"])
                P.tt("dve", cmpt, aff, mid.unsqueeze(1).broadcast_to([128, NT, 16]), ALU.is_ge, ["thr"], ["cmpt"])
                P.op("dve", lambda e: e.tensor_reduce(out=part, in_=cmpt.rearrange("p t e -> p e t"), axis=AX.X, op=ALU.add),
                     ["cmpt"], ["part"])
                pp, pk = nps()
                P.mm(pp[:, 0:16], onesf, part, True, True, ["part", "onesf"], [pk])
                P.ts("dve", ge, pp[:, 0:16], CAP - 0.5, None, ALU.is_ge, None, [pk], ["ge"])
                P.stt(lo, ge, ck, lo, ALU.mult, ALU.add, ["ge", "lo"], ["lo"])
            P.tt("dve", cmpt, aff, lo.unsqueeze(1).broadcast_to([128, NT, 16]), ALU.is_ge, ["lo"], ["cmpt"])
            cm2 = cmpt.rearrange("p t e -> p (t e)")
            pp1, pk1 = nps()
            P.mm(pp1[:, 0:NF], lstr, cm2, True, True, ["cmpt", "lstr"], [pk1])
            pp2, pk2 = nps()
            P.mm(pp2[:, 0:NF], onesf, cm2, True, True, ["cmpt", "onesf"], [pk2])
            P.cp("dve", p1.rearrange("p t e -> p (t e)"), pp1[:, 0:NF], [pk1], ["p1"])
            P.cp("dve", cA.rearrange("p t e -> p (t e)"), pp2[:, 0:NF], [pk2], ["cA"])
            cur, nxt, ck_, nk_ = cA, cB, "cA", "cB"
            s_ = 1
            while s_ < NT:
                P.tt("dve", nxt[:, s_:, :], cur[:, s_:, :], cur[:, 0:NT - s_, :], ALU.add, [ck_], [nk_])
                P.cp("dve", nxt[:, 0:s_, :], cur[:, 0:s_, :], [ck_], [nk_])
                cur, nxt, ck_, nk_ = nxt, cur, nk_, ck_
                s_ *= 2
            P.tt("dve", p1, p1, cur, ALU.add, ["p1", ck_], ["p1"])
            P.tt("dve", p1.rearrange("p t e -> p (t e)"), p1.rearrange("p t e -> p (t e)"), pp2[:, 0:NF], ALU.subtract,
                 ["p1", pk2], ["p1"])
            P.ts("dve", cmpt, cmpt, -1.0e6, 1.0e6, ALU.mult, ALU.add, ["cmpt"], ["cmpt"])
            P.tt("dve", p1, p1, cmpt, ALU.add, ["p1", "cmpt"], ["p1"])
            Rf = Rt.rearrange("p t e k -> p (t e) k")
            P.cp("dve", Rt[:, :, :, 0], tpi[:, :, 0:1].broadcast_to([128, NT, 16]), ["tpi"], ["Rt"])
            P.cp("dve", Rt[:, :, :, 1], tpi[:, :, 1:2].broadcast_to([128, NT, 16]), ["tpi"], ["Rt"])
            P.cp("dve", ahi, aff, [], ["ahi"])
            P.tt("dve", res1, aff, ahi, ALU.subtract, ["ahi"], ["res1"])
            P.cp("dve", amid, res1, ["res1"], ["amid"])
            P.tt("dve", res1, res1, amid, ALU.subtract, ["res1", "amid"], ["res1"])
            P.cp("dve", Rt[:, :, :, 2], ahi, ["ahi"], ["Rt"])
            P.cp("dve", Rt[:, :, :, 3], amid, ["amid"], ["Rt"])
            P.cp("dve", Rt[:, :, :, 4], res1, ["res1"], ["Rt"])

            def list_oh(e_, t):
                P.ts("dve", oh[t % 4], iof, p1[:, t, e_:e_ + 1], None, ALU.is_equal, None, ["iof", "p1"], [("oh", t % 4)])

            def list_mm(e_, t):
                o = oh[t % 4]
                for g in range(NG):
                    P.mm(psl[:, g * 8:g * 8 + 5], o[:, g * 128:(g + 1) * 128], Rt[:, t, e_, :], t == 0 and g == 0,
                         t == NT - 1 and g == NG - 1, [("oh", t % 4), "Rt"], ["psl"])

            def list_step(e_, t):
                list_oh(e_, t)
                list_mm(e_, t)

            def list_fin(e_):
                b = e_ % 2
                P.cp("dve", lst, psl[:, 0:NG * 8].rearrange("p (g k) -> p g k", k=8)[:, :, 0:5], ["psl"], ["lst"])
                P.stt(idf, lst[:, :, 0], 128.0, lst[:, :, 1], ALU.mult, ALU.add, ["lst"], ["idf"])
                P.cp("dve", ids2[b], idf, ["idf"], [("ids", b)])
                P.tt("dve", vals2[b], lst[:, :, 2], lst[:, :, 3], ALU.add, ["lst"], [("vals", b)])
                P.tt("dve", vals2[b], vals2[b], lst[:, :, 4], ALU.add, ["lst", ("vals", b)], [("vals", b)])
                for g in range(NG):
                    P.dma("pool", lambda e, g=g, b=b: e.indirect_dma_start(
                        out=xgs2[b][:, g, :], out_offset=None, in_=h2rows,
                        in_offset=bass.IndirectOffsetOnAxis(ap=ids2[b][:, g:g + 1], axis=0),
                        bounds_check=P.reg(e, S - 1), oob_is_err=False), [("ids", b)], [("xgs", b)])

            def build_lists(e_):
                for t in range(NT):
                    list_step(e_, t)
                list_fin(e_)

            def sadd(e_):
                ids = ids2[e_ % 2]
                for g in range(NG):
                    P.dma("pool", lambda e, g=g, ids=ids: e.indirect_dma_start(
                        out=xs, out_offset=bass.IndirectOffsetOnAxis(ap=ids[:, g:g + 1], axis=0),
                        in_=ysb[:, g, :], in_offset=None, bounds_check=P.reg(e, S - 1), oob_is_err=True, compute_op=ALU.add),
                        [("ysb", g), ("ids", e_ % 2)], ["xs"])

            build_lists(0)
            tri = 0
            for e_ in range(16):
                b = e_ % 2
                xgs, vals = xgs2[b], vals2[b]
                for g in range(NG):
                    pt = ptr[tri % 2]
                    for c in range(8):
                        P.tr(pt[:, c * 128:(c + 1) * 128], xgs[:, g, c * 128:(c + 1) * 128], identb, [("xgs", b), "identb"],
                             [("ptr", tri % 2)])
                    P.cp("act" if g % 2 else "dve", xgT[:, :, g * 128:(g + 1) * 128], pt.rearrange("p (c n) -> p c n", c=8),
                         [("ptr", tri % 2)], ["xgT"])
                    tri += 1
                if e_ > 0:
                    sadd(e_ - 1)
                for fc in range(8):
                    if e_ + 1 < 16:
                        for t in range(fc * NT // 8, (fc + 1) * NT // 8):
                            list_oh(e_ + 1, t)
                    pa, pka = nps()
                    pb_, pkb = nps()
                    for c in range(8):
                        P.mm(pa[:, 0:CAP], w1[e_ % 3][:, c, fc * 128:(fc + 1) * 128], xgT[:, c, :], c == 0, c == 7,
                             ["xgT", ("w1", e_ % 3, c)], [pka])
                    for c in range(8):
                        P.mm(pb_[:, 0:CAP], w3[e_ % 3][:, c, fc * 128:(fc + 1) * 128], xgT[:, c, :], c == 0, c == 7,
                             ["xgT", ("w3", e_ % 3, c)], [pkb])
                    P.act(s1, pa[:, 0:CAP], AF.Silu, [pka], ["s1"])
                    P.tt("dve", aT[:, fc, :], s1, pb_[:, 0:CAP], ALU.mult, ["s1", pkb], ["aT"])
                    if e_ + 1 < 16:
                        for t in range(fc * NT // 8, (fc + 1) * NT // 8):
                            list_mm(e_ + 1, t)
                if e_ + 1 < 16:
                    list_fin(e_ + 1)
                if e_ + 3 < 16:
                    wload13(e_ + 3)
                for g in range(NG):
                    for hf in range(2):
                        pp, pk = nps()
                        for c in range(8):
                            P.mm(pp, aT[:, c, g * 128:(g + 1) * 128], w2[b][:, c, hf * 512:(hf + 1) * 512], c == 0, c == 7,
                                 ["aT", ("w2", b, c)], [pk])
                        P.ts("dve", ysb[:, g, hf * 512:(hf + 1) * 512], pp, vals[:, g:g + 1], None, ALU.mult, None,
                             [pk, ("vals", b), "p1"], [("ysb", g)])
                if e_ + 2 < 16:
                    wload2(e_ + 2)
            sadd(15)
            P.flush()

        with ExitStack() as es:
            def sb(name, shape, dt):
                return es.enter_context(nc.sbuf_tensor(f"p6_{l}_{name}", list(shape), dt)).ap()

            def psb(name, shape=(128, 512), dt=F32):
                return es.enter_context(nc.psum_tensor(f"p6_{l}_{name}", list(shape), dt)).ap()

            wg = sb("wg", [128, 8, D], BF16)
            wp = sb("wp", [128, 2, D], BF16)
            gple = sb("gple", [128, D], F32)
            gfin = sb("gfin", [128, D], F32)
            xb2 = [sb(f"xblk{i}", [128, 4, D], F32) for i in range(2)]
            pb2 = [sb(f"pblk{i}", [128, 4, 256], F32) for i in range(2)]
            pbf2 = [sb(f"pbf{i}", [128, 4, 256], BF16) for i in range(2)]
            junk = sb("junk", [128, D], BF16)
            ss2 = [sb(f"ss{i}", [128, 4], F32) for i in range(2)]
            rstd2 = [sb(f"rstd{i}", [128, 4], F32) for i in range(2)]
            ssf = sb("ssf", [128, 4], F32)
            rstdf = sb("rstdf", [128, 4], F32)
            h2b = [sb(f"h{i}", [128, 4, D], BF16) for i in range(2)]
            hT = sb("hT", [128, 8, 512], BF16)
            pT = sb("pT", [128, 2, 512], BF16)
            sg = sb("sg", [128, 512], F32)
            sg1 = sb("sg1", [128, 512], F32)
            yb = sb("yb", [128, 4, D], F32)
            ps = [psb(f"ps{i}") for i in range(6)]
            pst = [psb(f"pst{i}", (128, 1024), BF16) for i in range(2)]
            psi = [0]

            def nps():
                i = psi[0] % 6
                psi[0] += 1
                return ps[i], ("ps", i)

            for c in range(8):
                P.ld("pool", wg[:, c, :], ple_gate_w[l, c * 128:(c + 1) * 128, :], [], [("wg", c)])
            for c in range(2):
                P.ld("pool", wp[:, c, :], ple_w[l, c * 128:(c + 1) * 128, :], [], [("wp", c)])
            P.ld("sp", gple, norm_ple[l].partition_broadcast(128), [], ["gple"])
            P.ld("sp", gfin, norm_final[0].partition_broadcast(128), [], ["gfin"])
            last = (l == L - 1)

            def pro_a(blk):
                i = blk % 2
                tok0 = blk * 512
                xblk, pblk, pbf, ss, rstd, h = xb2[i], pb2[i], pbf2[i], ss2[i], rstd2[i], h2b[i]
                P.ld("sp", xblk, rows_view(xs, tok0, 4), [], [("xblk", i)])
                P.ld("sp", pblk, rows_view(p_in[l], tok0, 4), [], [("pblk", i)])
                for t in range(4):
                    P.act(junk, xblk[:, t, :], AF.Square, [("xblk", i)], ["junk", ("ss", i)], accum_out=ss[:, t:t + 1])
                P.ts("dve", rstd, ss, 1.0 / D, EPS, ALU.mult, ALU.add, [("ss", i)], [("rstd", i)])
                P.op("act", lambda e: e.sqrt(out=rstd, in_=rstd), [("rstd", i)], [("rstd", i)])
                P.op("dve", lambda e: e.reciprocal(out=rstd, in_=rstd), [("rstd", i)], [("rstd", i)])
                P.cp("pool", pbf, pblk, [("pblk", i)], [("pbf", i)])
                for t in range(4):
                    P.stt(h[:, t, :], xblk[:, t, :], rstd[:, t:t + 1], gple, ALU.mult, ALU.mult,
                          [("xblk", i), ("rstd", i), "gple"], [("h", i, t)])

            def pro_b(blk):
                i = blk % 2
                h, pbf = h2b[i], pbf2[i]
                for t in range(4):
                    pt = pst[t % 2]
                    for c in range(8):
                        P.tr(pt[:, c * 128:(c + 1) * 128], h[:, t, c * 128:(c + 1) * 128], identb,
                             [("h", i, t), "identb"], [("pst", t % 2)])
                    P.cp("act" if t % 2 else "dve", hT[:, :, t * 128:(t + 1) * 128],
                         pt.rearrange("p (c n) -> p c n", c=8), [("pst", t % 2)], ["hT"])
                for t in range(4):
                    pt = pst[t % 2]
                    for c in range(2):
                        P.tr(pt[:, c * 128:(c + 1) * 128], pbf[:, t, c * 128:(c + 1) * 128], identb,
                             [("pbf", i), "identb"], [("pst", t % 2)])
                    P.cp("act" if t % 2 else "dve", pT[:, :, t * 128:(t + 1) * 128],
                         pt[:, 0:256].rearrange("p (c n) -> p c n", c=2), [("pst", t % 2)], ["pT"])

            pro_a(0)
            pro_b(0)
            for blk in range(NB):
                tok0 = blk * 512
                bi = blk % 2
                xblk = xb2[bi]
                xk = ("xblk", bi)
                if blk + 1 < NB:
                    pro_a(blk + 1)
                for t in range(4):
                    for hf in range(2):
                        pg, pkg = nps()
                        pl, pkl = nps()
                        for c in range(8):
                            P.mm(pg, hT[:, c, t * 128:(t + 1) * 128], wg[:, c, hf * 512:(hf + 1) * 512], c == 0, c == 7,
                                 ["hT", ("wg", c)], [pkg])
                        for c in range(2):
                            P.mm(pl, pT[:, c, t * 128:(t + 1) * 128], wp[:, c, hf * 512:(hf + 1) * 512], c == 0, c == 1,
                                 ["pT", ("wp", c)], [pkl])
                        sg_ = sg if hf == 0 else sg1
                        sk = ("sg", hf)
                        P.act(sg_, pg, AF.Sigmoid, [pkg], [sk])
                        P.tt("dve", sg_, sg_, pl, ALU.mult, [sk, pkl], [sk])
                        P.tt("pool", xblk[:, t, hf * 512:(hf + 1) * 512], xblk[:, t, hf * 512:(hf + 1) * 512], sg_, ALU.add,
                             [sk, xk], [xk])
                if blk + 1 < NB:
                    pro_b(blk + 1)
                if not last:
                    P.ld("sp", rows_view(xs, tok0, 4), xblk, [xk], ["xs"])
                else:
                    for t in range(4):
                        P.act(junk, xblk[:, t, :], AF.Square, [xk], ["junk", "ssf"], accum_out=ssf[:, t:t + 1])
                    P.ts("dve", rstdf, ssf, 1.0 / D, EPS, ALU.mult, ALU.add, ["ssf"], ["rstdf"])
                    P.op("act", lambda e: e.sqrt(out=rstdf, in_=rstdf), ["rstdf"], ["rstdf"])
                    P.op("dve", lambda e: e.reciprocal(out=rstdf, in_=rstdf), ["rstdf"], ["rstdf"])
                    for t in range(4):
                        P.stt(yb[:, t, :], xblk[:, t, :], rstdf[:, t:t + 1], gfin, ALU.mult, ALU.mult,
                              [xk, "rstdf", "gfin"], ["yb"])
                    P.ld("sp", rows_view(y_out, tok0, 4), yb, ["yb"], ["y"])
            P.flush()
    return nc


def _host_layout(inputs, S, L):
    f = np.float32
    w_in = np.asarray(inputs["w_in"], f)[:L]
    kr = 2176
    swap = np.concatenate([np.arange(8, 16), np.arange(0, 8), np.arange(24, 32), np.arange(16, 24)])
    w_in_ext = np.concatenate([w_in, w_in[:, :, 2112:2176], w_in[:, :, kr + swap]], axis=2)
    wq = np.asarray(inputs["mla_wq_up"], f)[:L].reshape(L, 384, 8, 96)
    wq_ext = np.concatenate([wq, wq[..., 0:64], wq[..., 64 + swap]], axis=3).reshape(L, 384, 8 * 192)
    wkv = np.asarray(inputs["mla_wkv_up"], f)[:L].reshape(L, 256, 8, 128)
    wkv_re = np.concatenate([wkv[..., 0:64].reshape(L, 256, 512), wkv[..., 64:128].reshape(L, 256, 512)], axis=2)
    rpb = np.asarray(inputs["na_rpb"], f)[:L]
    kc = np.arange(64)[:, None]
    qc = np.arange(64)[None, :]
    dc = np.clip(kc - qc + 15, 0, 30)
    tz = np.empty((L, 128, 8, 14, 64), f)
    for krel in range(2):
        for d in range(14):
            tz[:, krel * 64:(krel + 1) * 64, :, d, :] = np.transpose(rpb[:, :, d + krel, :][:, :, dc], (0, 2, 1, 3))
    col_start = np.clip(np.arange(64) - 8, 0, 48)[None, :]
    valid = (kc >= col_start) & (kc < col_start + 16)
    colmask = np.where(valid, 0.0, MASKV).astype(f)
    colmask = np.concatenate([colmask, colmask], axis=0)
    t = np.arange(S)
    freqs = (1.0 / (10000.0 ** (np.arange(0, 16, 2, dtype=f) / f(16)))).astype(f)
    cos = np.empty((32, S), f)
    sin = np.empty((32, S), f)
    for j in range(32):
        pos = (t // 64) if j < 16 else (t % 64)
        jj = j % 16
        ang = pos.astype(f) * freqs[jj % 8]
        cos[j] = np.cos(ang)
        sin[j] = np.sin(ang) * (-1.0 if jj < 8 else 1.0)
    rope = np.stack([cos, sin]).astype(f)
    shared = {
        "norm_mix": inputs["norm_mix"][:L], "w_in_ext": w_in_ext, "tz": tz.reshape(L, 128, -1), "colmask": colmask,
        "rope": rope, "mla_q_norm": inputs["mla_q_norm"][:L], "wq_ext": wq_ext, "mla_kv_norm": inputs["mla_kv_norm"][:L],
        "wkv_re": wkv_re, "w_na_o": inputs["w_na_o"][:L], "w_mla_o": inputs["w_mla_o"][:L], "w_out": inputs["w_out"][:L],
        "norm_moe": inputs["norm_moe"][:L], "w_router": inputs["w_router"][:L], "moe_w1": inputs["moe_w1"][:L],
        "moe_w3": inputs["moe_w3"][:L], "moe_w2": inputs["moe_w2"][:L], "norm_ple": inputs["norm_ple"][:L],
        "ple_gate_w": inputs["ple_gate_w"][:L], "ple_w": inputs["ple_w"][:L],
        "norm_final": np.asarray(inputs["norm_final"], f).reshape(1, D),
    }
    return {k: np.ascontiguousarray(np.asarray(v, f)) for k, v in shared.items()}


def kernel(**inputs):
    x = np.asarray(inputs["x"], np.float32)
    p = np.asarray(inputs["p"], np.float32)
    B, S, _ = x.shape
    L = p.shape[0]
    nc = build(S, L)
    shared = _host_layout(inputs, S, L)
    in_maps = []
    for b in range(B):
        m = dict(shared)
        m["x"] = np.ascontiguousarray(x[b])
        m["p"] = np.ascontiguousarray(p[:, b])
        in_maps.append(m)
    res = run_bass_kernel_spmd(nc, in_maps, core_ids=list(range(B)))
    return np.stack([np.asarray(r["y"], np.float32) for r in res.results], axis=0)
```
